# Optimizing a Trainium2 kernel written in Bass

```python
import jax, jax.numpy as jnp
from jax import lax
import numpy as np

D_MODEL = 1024
BATCH = 8
SEQ = 4096
DEPTH = 4

HEAD_DIM = D_MODEL // 16
SB_HEADS = 6
ML_HEADS = 6
SG_GROUPS = 4
SB_WIDTH = SB_HEADS * HEAD_DIM
ML_WIDTH = ML_HEADS * HEAD_DIM
SG_WIDTH = SG_GROUPS * HEAD_DIM
MIX_HEADS = SB_HEADS + ML_HEADS + SG_GROUPS
MIX_WIDTH = SB_WIDTH + ML_WIDTH + SG_WIDTH
IN_SIZES = (SB_WIDTH, SB_WIDTH, SB_WIDTH, 2 * ML_WIDTH, ML_WIDTH, ML_WIDTH, ML_HEADS, ML_HEADS, 2 * SG_WIDTH)
IN_WIDTH = sum(IN_SIZES)
Q_BLOCK = 128
ML_CHUNK = 128
SG_CHUNK = 128
CONV_WIDTH = 4
PEER_HEADS = 8
PEER_NKEYS = 128
PEER_EXPERTS = PEER_NKEYS * PEER_NKEYS
PEER_SUBDIM = 128
PEER_TOPK = 16
PEER_BLOCK = 128
NORM_EPS = 1e-6

kernel_name = "hybrid_sb_mlstm_sgmlp_peer"


def rms_norm(x, g):
    x32 = x.astype(jnp.float32)
    return x32 * lax.rsqrt(jnp.mean(x32 * x32, axis=-1, keepdims=True) + NORM_EPS) * g.astype(jnp.float32)


def split_heads(t, n_heads):
    b, s, _ = t.shape
    return t.reshape(b, s, n_heads, HEAD_DIM).transpose(0, 2, 1, 3)


def causal_conv(t, w, b):
    s = t.shape[1]
    tp = jnp.pad(t, ((0, 0), (CONV_WIDTH - 1, 0), (0, 0)))
    out = b.astype(jnp.float32)
    for j in range(CONV_WIDTH):
        out = out + tp[:, j:j + s] * w[j]
    return out


def stick_breaking_attention(q, k, v):
    s_len = q.shape[2]
    scale = HEAD_DIM ** -0.5
    outs = []
    for blk in range(s_len // Q_BLOCK):
        start = blk * Q_BLOCK
        end = start + Q_BLOCK
        z = jnp.einsum('bhqd,bhkd->bhqk', q[:, :, start:end], k[:, :, :end]) * scale
        before = jnp.arange(end)[None, :] < (start + jnp.arange(Q_BLOCK))[:, None]
        log_keep = jnp.where(before, -jax.nn.softplus(z), 0.0)
        log_keep_after = lax.cumsum(log_keep, axis=3, reverse=True) - log_keep
        w = jnp.where(before, jnp.exp(jax.nn.log_sigmoid(z) + log_keep_after), 0.0)
        outs.append(jnp.einsum('bhqk,bhkd->bhqd', w, v[:, :, :end]))
    return jnp.concatenate(outs, axis=2)


def mlstm_chunkwise(q, k, v, i_pre, f_pre):
    b, h, s_len, d = q.shape
    nc = s_len // ML_CHUNK
    log_f = jax.nn.log_sigmoid(f_pre)

    def chunks(t):
        return jnp.moveaxis(t.reshape(b, h, nc, ML_CHUNK, *t.shape[3:]), 2, 0)

    tril = jnp.tril(jnp.ones((ML_CHUNK, ML_CHUNK), dtype=bool))

    def step(carry, inp):
        c_mat, n_vec, m = carry
        qc, kc, vc, ic, lfc = inp
        bcum = jnp.cumsum(lfc, axis=-1)
        dmat = jnp.where(tril, bcum[..., :, None] - bcum[..., None, :] + ic[..., None, :], -jnp.inf)
        m_inter = bcum + m[..., None]
        m_t = jnp.maximum(m_inter, jnp.max(dmat, axis=-1))
        inter_scale = jnp.exp(m_inter - m_t)
        qk = jnp.einsum('bhtd,bhsd->bhts', qc, kc) * jnp.exp(dmat - m_t[..., None])
        num = jnp.einsum('bhts,bhsd->bhtd', qk, vc) + inter_scale[..., None] * jnp.einsum('bhtd,bhde->bhte', qc, c_mat)
        den = jnp.sum(qk, axis=-1) + inter_scale * jnp.einsum('bhtd,bhd->bht', qc, n_vec)
        h_out = num / jnp.maximum(jnp.abs(den), jnp.exp(-m_t))[..., None]
        b_last = bcum[..., -1]
        g_s = b_last[..., None] - bcum + ic
        m_new = jnp.maximum(b_last + m, jnp.max(g_s, axis=-1))
        w_s = jnp.exp(g_s - m_new[..., None])
        decay = jnp.exp(b_last + m - m_new)
        c_new = decay[..., None, None] * c_mat + jnp.einsum('bhs,bhsd,bhse->bhde', w_s, kc, vc)
        n_new = decay[..., None] * n_vec + jnp.einsum('bhs,bhsd->bhd', w_s, kc)
        return (c_new, n_new, m_new), h_out

    init = (jnp.zeros((b, h, d, d), jnp.float32), jnp.zeros((b, h, d), jnp.float32), jnp.zeros((b, h), jnp.float32))
    _, hs = lax.scan(step, init, (chunks(q), chunks(k), chunks(v), chunks(i_pre), chunks(log_f)))
    return jnp.moveaxis(hs, 0, 2).reshape(b, h, s_len, d)


def spatial_gating(u, v, w_s, b_s):
    b, s_len, _ = u.shape
    nc = s_len // SG_CHUNK
    tril = jnp.tril(jnp.ones((SG_CHUNK, SG_CHUNK), dtype=jnp.float32))
    v5 = v.reshape(b, nc, SG_CHUNK, SG_GROUPS, HEAD_DIM)
    gate = jnp.einsum('gts,bcsgd->bctgd', w_s.astype(jnp.float32) * tril, v5) + b_s.astype(jnp.float32).T[:, :, None]
    return (u.reshape(b, nc, SG_CHUNK, SG_GROUPS, HEAD_DIM) * gate).reshape(b, s_len, SG_GROUPS, HEAD_DIM)


def mixing_sublayer(xn, w_in, sb_qn_g, sb_kn_g, ml_conv_w, ml_conv_b, ml_i_b, ml_f_b,
                    sg_vn_g, sg_w, sg_b, out_g, w_out):
    b, s_len, _ = xn.shape
    z = jnp.matmul(xn, w_in).astype(jnp.float32)
    cuts = [int(c) for c in np.cumsum(IN_SIZES)[:-1]]
    sb_q, sb_k, sb_v, ml_qk, ml_v, ml_o, ml_i, ml_f, sg_uv = jnp.split(z, cuts, axis=-1)
    head_g = out_g.astype(jnp.float32).reshape(MIX_HEADS, HEAD_DIM)

    q = rms_norm(split_heads(sb_q, SB_HEADS), sb_qn_g)
    k = rms_norm(split_heads(sb_k, SB_HEADS), sb_kn_g)
    y_sb = stick_breaking_attention(q, k, split_heads(sb_v, SB_HEADS)).transpose(0, 2, 1, 3)
    y_sb = rms_norm(y_sb, head_g[:SB_HEADS])

    qk = jax.nn.silu(causal_conv(ml_qk, ml_conv_w.astype(jnp.float32), ml_conv_b))
    mq = split_heads(qk[..., :ML_WIDTH], ML_HEADS)
    mk = split_heads(qk[..., ML_WIDTH:], ML_HEADS) * (HEAD_DIM ** -0.5)
    i_pre = (ml_i + ml_i_b.astype(jnp.float32)).transpose(0, 2, 1)
    f_pre = (ml_f + ml_f_b.astype(jnp.float32)).transpose(0, 2, 1)
    y_ml = mlstm_chunkwise(mq, mk, split_heads(ml_v, ML_HEADS), i_pre, f_pre).transpose(0, 2, 1, 3)
    o_gate = jax.nn.sigmoid(ml_o).reshape(b, s_len, ML_HEADS, HEAD_DIM)
    y_ml = rms_norm(y_ml, head_g[SB_HEADS:SB_HEADS + ML_HEADS]) * o_gate

    uv = jax.nn.gelu(sg_uv)
    u = uv[..., :SG_WIDTH]
    v = rms_norm(uv[..., SG_WIDTH:].reshape(b, s_len, SG_GROUPS, HEAD_DIM),
                 sg_vn_g.reshape(SG_GROUPS, HEAD_DIM)).reshape(b, s_len, SG_WIDTH)
    y_sg = rms_norm(spatial_gating(u, v, sg_w, sg_b), head_g[SB_HEADS + ML_HEADS:])

    y = jnp.concatenate([y_sb, y_ml, y_sg], axis=2).reshape(b, s_len, MIX_WIDTH)
    return jnp.matmul(y.astype(xn.dtype), w_out)


def peer_ffn(xn, wq, sub_keys, u_tab, v_tab):
    b, s_len, d = xn.shape
    t = b * s_len
    xt = xn.reshape(t, d)
    q = jnp.matmul(xt, wq).astype(jnp.float32).reshape(t, PEER_HEADS, 2, PEER_SUBDIM)
    scores = jnp.einsum('thcd,hcnd->thcn', q, sub_keys.astype(jnp.float32))
    sv, si = lax.top_k(scores, PEER_TOPK)
    cand_s = (sv[:, :, 0, :, None] + sv[:, :, 1, None, :]).reshape(t, PEER_HEADS, PEER_TOPK * PEER_TOPK)
    cand_i = (si[:, :, 0, :, None] * PEER_NKEYS + si[:, :, 1, None, :]).reshape(t, PEER_HEADS, PEER_TOPK * PEER_TOPK)
    best_s, best_p = lax.top_k(cand_s, PEER_TOPK)
    idx = jnp.take_along_axis(cand_i, best_p, axis=-1)
    gate = jax.nn.softmax(best_s, axis=-1)
    nb = t // PEER_BLOCK

    def expert_block(args):
        xb, ib, gb = args
        act = jnp.einsum('td,thkd->thk', xb.astype(jnp.float32), u_tab[ib].astype(jnp.float32))
        wgt = jax.nn.gelu(act) * gb
        return jnp.einsum('thk,thkd->td', wgt, v_tab[ib].astype(jnp.float32))

    y = lax.map(expert_block, (xt.reshape(nb, PEER_BLOCK, d),
                               idx.reshape(nb, PEER_BLOCK, PEER_HEADS, PEER_TOPK),
                               gate.reshape(nb, PEER_BLOCK, PEER_HEADS, PEER_TOPK)))
    return y.reshape(b, s_len, d)


def setup_inputs(seed: int = 0) -> dict:
    key = jax.random.key(seed)
    ks = jax.random.split(key, 20)
    f32 = jnp.float32
    nrm = lambda k, shape, scale: jax.random.normal(k, shape, f32) * scale
    return {
        "x": nrm(ks[0], (BATCH, SEQ, D_MODEL), 1.0),
        "norm1_g": 1.0 + nrm(ks[1], (DEPTH, D_MODEL), 0.02),
        "w_in": nrm(ks[2], (DEPTH, D_MODEL, IN_WIDTH), D_MODEL ** -0.5),
        "sb_qn_g": 1.0 + nrm(ks[3], (DEPTH, HEAD_DIM), 0.02),
        "sb_kn_g": 1.0 + nrm(ks[4], (DEPTH, HEAD_DIM), 0.02),
        "ml_conv_w": nrm(ks[5], (DEPTH, CONV_WIDTH, 2 * ML_WIDTH), CONV_WIDTH ** -0.5),
        "ml_conv_b": nrm(ks[6], (DEPTH, 2 * ML_WIDTH), 0.02),
        "ml_i_b": nrm(ks[7], (DEPTH, ML_HEADS), 0.1),
        "ml_f_b": jnp.linspace(3.0, 6.0, ML_HEADS, dtype=f32)[None, :] + nrm(ks[8], (DEPTH, ML_HEADS), 0.02),
        "sg_vn_g": 1.0 + nrm(ks[9], (DEPTH, SG_WIDTH), 0.02),
        "sg_w": nrm(ks[10], (DEPTH, SG_GROUPS, SG_CHUNK, SG_CHUNK), SG_CHUNK ** -0.5),
        "sg_b": 1.0 + nrm(ks[11], (DEPTH, SG_GROUPS, SG_CHUNK), 0.02),
        "out_g": 1.0 + nrm(ks[12], (DEPTH, MIX_WIDTH), 0.02),
        "w_out": nrm(ks[13], (DEPTH, MIX_WIDTH, D_MODEL), MIX_WIDTH ** -0.5),
        "norm2_g": 1.0 + nrm(ks[14], (DEPTH, D_MODEL), 0.02),
        "peer_wq": nrm(ks[15], (DEPTH, D_MODEL, PEER_HEADS * 2 * PEER_SUBDIM), D_MODEL ** -0.5),
        "peer_keys": nrm(ks[16], (DEPTH, PEER_HEADS, 2, PEER_NKEYS, PEER_SUBDIM), PEER_SUBDIM ** -0.5),
        "peer_u": nrm(ks[17], (DEPTH, PEER_EXPERTS, D_MODEL), D_MODEL ** -0.5),
        "peer_v": nrm(ks[18], (DEPTH, PEER_EXPERTS, D_MODEL), (PEER_HEADS * PEER_TOPK) ** -0.5),
    }


def reference(x, norm1_g, w_in, sb_qn_g, sb_kn_g, ml_conv_w, ml_conv_b, ml_i_b, ml_f_b,
              sg_vn_g, sg_w, sg_b, out_g, w_out, norm2_g, peer_wq, peer_keys, peer_u, peer_v):
    for l in range(DEPTH):
        xn = rms_norm(x, norm1_g[l]).astype(x.dtype)
        x = x + mixing_sublayer(xn, w_in[l], sb_qn_g[l], sb_kn_g[l], ml_conv_w[l], ml_conv_b[l],
                                ml_i_b[l], ml_f_b[l], sg_vn_g[l], sg_w[l], sg_b[l], out_g[l],
                                w_out[l]).astype(x.dtype)
        xn = rms_norm(x, norm2_g[l]).astype(x.dtype)
        x = x + peer_ffn(xn, peer_wq[l], peer_keys[l], peer_u[l], peer_v[l]).astype(x.dtype)
    return x
```

```python
from contextlib import ExitStack
import numpy as np
import ml_dtypes
import concourse.bass as bass
import concourse.mybir as mybir
from concourse.ap import AP
from concourse.bass_utils import run_bass_kernel_spmd

F32 = mybir.dt.float32
BF16 = mybir.dt.bfloat16
U32 = mybir.dt.uint32
ALU = mybir.AluOpType
AF = mybir.ActivationFunctionType
AX = mybir.AxisListType

D = 1024
NCORES = 8
EPS = 1e-6
NEG = -1.0e30
COLS_A = 2444
OQ, OK_, OV, OMV, OMO, OSG, OMI, OMF = 0, 384, 768, 1152, 1536, 1920, 2432, 2438


class Tile:
    __slots__ = ("ap", "keys")

    def __init__(self, ap, keys):
        self.ap = ap
        self.keys = tuple(keys)

    def __getitem__(self, k):
        return self.ap[k]


class Prog:
    ENGS = ("pe", "act", "dve", "pool", "sp")
    PAGE = 512

    def __init__(self, nc, es):
        self.nc = nc
        self.es = es
        self.ops = []
        self.nkey = 0
        self.arenas = {}
        self.dma_keys = {}

    def key(self):
        self.nkey += 1
        return ("k", self.nkey)

    def static(self, name, shape, dtype):
        t = self.es.enter_context(self.nc.sbuf_tensor("sb_" + name, list(shape), dtype))
        return Tile(t[:], [self.key()])

    def psum(self, name, shape, dtype):
        t = self.es.enter_context(self.nc.psum_tensor(name, list(shape), dtype))
        return Tile(t[:], [self.key()])

    def arena(self, name, nelem, dtype):
        t = self.es.enter_context(self.nc.sbuf_tensor(name, [128, nelem], dtype))
        self.arenas[name] = dict(t=t, n=nelem, off=0)

    def mark(self, name):
        return self.arenas[name]["off"]

    def reset(self, name, m):
        self.arenas[name]["off"] = m

    def alloc(self, name, shape):
        a = self.arenas[name]
        n = int(np.prod(shape))
        off = a["off"]
        npg = (n + self.PAGE - 1) // self.PAGE
        assert off + npg * self.PAGE <= a["n"], f"arena {name} overflow need {off + npg * self.PAGE} have {a['n']}"
        a["off"] = off + npg * self.PAGE
        ap = a["t"][:, off:off + n]
        if len(shape) == 2:
            ap = ap.rearrange("p (a b) -> p a b", a=shape[0])
        elif len(shape) == 3:
            ap = ap.rearrange("p (a b c) -> p a b c", a=shape[0], b=shape[1])
        keys = [(name, off // self.PAGE + i) for i in range(npg)]
        return Tile(ap, keys)

    def dkey(self, *k):
        return Tile(None, [("d",) + tuple(k)])

    def add(self, eng, fn, r=(), w=(), dma=None):
        import os
        if len(self.ops) >= int(os.environ.get("OPLIMIT", "100000000")):
            return
        rk = []
        for t in r:
            rk.extend(t.keys)
        wk = []
        for t in w:
            wk.extend(t.keys)
        self.ops.append([eng, fn, rk, wk, dma])

    def pos(self, name):
        import os
        if os.environ.get("SHOWPOS"):
            print("POS", name, len(self.ops), flush=True)

    def pe(self, fn, r=(), w=()):
        self.add("pe", fn, r, w)

    def act(self, fn, r=(), w=()):
        self.add("act", fn, r, w)

    def dve(self, fn, r=(), w=()):
        self.add("dve", fn, r, w)

    def pool(self, fn, r=(), w=()):
        self.add("pool", fn, r, w)

    def dma(self, q, key, out, in_, r=(), w=()):
        self.add(q, lambda e: e.dma_start(out=out, in_=in_), r, w, dma=key)

    def finalize(self):
        nc = self.nc
        ops = self.ops
        n = len(ops)
        lastw = {}
        readers = {}
        deps_all = [None] * n
        for k, (eng, fn, rk, wk, dma) in enumerate(ops):
            deps = set()
            for b in rk:
                d = lastw.get(b)
                if d is not None:
                    deps.add(d)
            for b in wk:
                d = lastw.get(b)
                if d is not None:
                    deps.add(d)
                rd = readers.get(b)
                if rd:
                    deps.update(rd)
            deps.discard(k)
            deps_all[k] = deps
            for b in rk:
                readers.setdefault(b, []).append(k)
            for b in wk:
                lastw[b] = k
                readers[b] = []
        waited = {e: {} for e in self.ENGS}
        need = [None] * n
        signal = [False] * n
        for k in range(n):
            eng = ops[k][0]
            wl = waited[eng]
            nd = {}
            for d in deps_all[k]:
                deng, _, _, _, ddma = ops[d]
                if ddma is not None:
                    pk = ("dma", ddma)
                    if wl.get(pk, -1) >= d:
                        continue
                    nd[pk] = max(nd.get(pk, -1), d)
                else:
                    if deng == "pe" and eng == "pe":
                        continue
                    pk = ("eng", deng)
                    if wl.get(pk, -1) >= d:
                        continue
                    nd[pk] = max(nd.get(pk, -1), d)
            for pk, d in nd.items():
                wl[pk] = max(wl.get(pk, -1), d)
                if pk[0] == "eng":
                    signal[d] = True
            need[k] = nd
        sem_eng = {e: self.es.enter_context(nc.semaphore("s_" + e)) for e in self.ENGS}
        cnt = {e: 0 for e in self.ENGS}
        val = [0] * n
        dma_cnt = {}
        dma_idx = {}
        for k in range(n):
            eng, _, _, _, dma = ops[k]
            if dma is not None:
                dma_cnt[dma] = dma_cnt.get(dma, 0) + 1
                dma_idx.setdefault(dma, []).append(k)
                val[k] = 16 * dma_cnt[dma]
            elif signal[k]:
                cnt[eng] += 1
                val[k] = cnt[eng]
        sem_dma = {dk: self.es.enter_context(nc.semaphore("d_%d" % i)) for i, dk in enumerate(dma_cnt)}
        import bisect
        per = {e: [] for e in self.ENGS}
        for k in range(n):
            per[ops[k][0]].append(k)
        final_waits = [(sem_dma[dk], 16 * c) for dk, c in dma_cnt.items()]
        self.stats = {e: len(per[e]) for e in self.ENGS}
        self.stats["signals"] = sum(signal)

        def emit(e, name):
            for k in per[name]:
                _, fn, _, _, dma = ops[k]
                for pk, d in need[k].items():
                    if pk[0] == "eng":
                        e.wait_ge(sem_eng[pk[1]], val[d])
                    else:
                        lst = dma_idx[pk[1]]
                        c = bisect.bisect_left(lst, k)
                        e.wait_ge(sem_dma[pk[1]], 16 * c)
                ins = fn(e)
                if dma is not None:
                    ins.then_inc(sem_dma[dma], 16)
                elif signal[k]:
                    ins.then_inc(sem_eng[name], 1)
            if name == "pool":
                for s, v in final_waits:
                    e.wait_ge(s, v)
                for en in self.ENGS:
                    if en != "pool" and cnt[en] > 0:
                        e.wait_ge(sem_eng[en], cnt[en])

        block = self.es.enter_context(nc.Block())

        @block.sync
        def _(e):
            emit(e, "sp")

        @block.tensor
        def _(e):
            emit(e, "pe")

        @block.scalar
        def _(e):
            emit(e, "act")

        @block.vector
        def _(e):
            emit(e, "dve")

        @block.gpsimd
        def _(e):
            emit(e, "pool")


def bcast_rows(dram_ap_1d_tensor, offset, n):
    return AP(dram_ap_1d_tensor, offset, [[0, 128], [1, n]])


def MM(P, ot, oap, lt, lap, rt, rap, start=True, stop=True):
    P.pe(lambda e: e.matmul(oap, lhsT=lap, rhs=rap, start=start, stop=stop), r=[lt, rt], w=[ot])


def TR(P, ot, oap, it, iap, idt, idap):
    P.pe(lambda e: e.transpose(oap, iap, idap), r=[it, idt], w=[ot])


def ACTV(P, ot, oap, it, iap, func, bias=0.0, scale=1.0, r=(), accum=None, w=()):
    if accum is None:
        P.act(lambda e: e.activation(out=oap, in_=iap, func=func, bias=bias, scale=scale), r=[it] + list(r), w=[ot] + list(w))
    else:
        P.act(lambda e: e.activation(out=oap, in_=iap, func=func, bias=bias, scale=scale, accum_out=accum),
              r=[it] + list(r), w=[ot] + list(w))


def TT(P, eng, ot, oap, at, aap, bt, bap, op):
    P.add(eng, lambda e: e.tensor_tensor(out=oap, in0=aap, in1=bap, op=op), r=[at, bt], w=[ot])


def TS(P, eng, ot, oap, at, aap, s1, s2, op0, op1=None, r=()):
    if op1 is None:
        P.add(eng, lambda e: e.tensor_scalar(out=oap, in0=aap, scalar1=s1, scalar2=None, op0=op0), r=[at] + list(r), w=[ot])
    else:
        P.add(eng, lambda e: e.tensor_scalar(out=oap, in0=aap, scalar1=s1, scalar2=s2, op0=op0, op1=op1),
              r=[at] + list(r), w=[ot])


def STT(P, ot, oap, at, aap, sc, bt, bap, op0, op1, r=()):
    P.dve(lambda e: e.scalar_tensor_tensor(out=oap, in0=aap, scalar=sc, in1=bap, op0=op0, op1=op1),
          r=[at, bt] + list(r), w=[ot])


def CP(P, eng, ot, oap, it, iap):
    if eng == "act":
        P.act(lambda e: e.copy(out=oap, in_=iap), r=[it], w=[ot])
    else:
        P.add(eng, lambda e: e.tensor_copy(out=oap, in_=iap), r=[it], w=[ot])


def RED(P, ot, oap, it, iap, op, axis=None):
    axis = AX.X if axis is None else axis
    P.dve(lambda e: e.tensor_reduce(out=oap, in_=iap, axis=axis, op=op), r=[it], w=[ot])


def bc(ap, axis, n):
    a = ap.unsqueeze(axis)
    shp = list(a.shape)
    shp[axis] = n
    return a.broadcast_to(shp)


C_IDF, C_IOTA, C_IOTA16, C_THR, C_TRI, C_NTRI, C_NONE, C_MASK4 = 0, 128, 256, 272, 288, 416, 544, 672
NCST = 672 + 2048


def make_consts():
    c = np.zeros((128, NCST), np.float32)
    c[:, C_IDF:C_IDF + 128] = np.eye(128, dtype=np.float32)
    c[:, C_IOTA:C_IOTA + 128] = np.arange(128, dtype=np.float32)[None, :]
    c[:, C_IOTA16:C_IOTA16 + 16] = np.arange(16, dtype=np.float32)[None, :]
    c[:, C_THR:C_THR + 16] = (16.0 * np.arange(1, 17, dtype=np.float32))[None, :]
    j = np.arange(128)[:, None]
    t = np.arange(128)[None, :]
    c[:, C_TRI:C_TRI + 128] = (j <= t).astype(np.float32)
    c[:, C_NTRI:C_NTRI + 128] = -(j >= t).astype(np.float32)
    c[:, C_NONE:C_NONE + 128] = -1.0
    q = np.arange(512)[None, :]
    for r in range(4):
        c[:, C_MASK4 + r * 512:C_MASK4 + (r + 1) * 512] = ((128 * r + j) < q).astype(np.float32)
    return c


def build(S, L, phases=("mix", "ffn"), dbg=False):
    NT = S // 128
    nc = bass.Bass("TRN2", target_bir_lowering=False)
    es = ExitStack()
    P = Prog(nc, es)

    def din(name, shape):
        return nc.dram_tensor(name, list(shape), F32, kind="ExternalInput")

    x_d = din("x", [S, D])
    cst_d = din("cst", [128, NCST])
    gvec_d = din("gvec", [L, 3, D])
    svec_d = din("svec", [L, 396])
    convw_d = din("convw", [L, 128, 30])
    sgw_d = din("sgw", [L, 128, 512])
    sgb_d = din("sgb", [L, 128, 4])
    wina_d = din("wina", [L, 128, 8 * COLS_A])
    winb_d = din("winb", [L, 128, 8 * 768])
    wout_d = din("wout", [L, 128, 8 * 1024])
    wq_d = din("wq", [L, 128, 8 * 2048])
    keys_d = din("pkeys", [L, 128, 16 * 128])
    if "ffn" in phases:
        ut_d = din("ut", [L, 128, 128, 1024])
        vt_d = din("vt", [L, 128, 128, 1024])
    out_d = nc.dram_tensor("out", [S, D], F32, kind="ExternalOutput")
    ubf_d = nc.dram_tensor("ubf", [L, 128, 128, 1024], BF16, kind="Internal")
    vbf_d = nc.dram_tensor("vbf", [L, 128, 128, 1024], BF16, kind="Internal")
    yms_d = nc.dram_tensor("yms", [S, 640], BF16, kind="Internal")
    dbg_d = nc.dram_tensor("dbg", [S, D], F32, kind="ExternalOutput") if dbg else None

    P.arena("abf", 136 * 512, BF16)
    P.arena("af", 16 * 512, F32)

    ps = [P.psum("ps%d" % i, [128, 512], F32) for i in range(7)]
    psT = P.psum("psT", [128, 1024], BF16)
    psTf = Tile(psT.ap.bitcast(F32), psT.keys)

    cstf = P.static("cstf", [128, C_MASK4], F32)
    P.dma("sp", "cst", cstf[:, :], cst_d.ap()[:, 0:C_MASK4], w=[cstf])
    identb = P.static("identb", [128, 128], BF16)
    iotab = P.static("iotab", [128, 128], BF16)
    CP(P, "dve", identb, identb[:, :], cstf, cstf[:, C_IDF:C_IDF + 128])
    CP(P, "dve", iotab, iotab[:, :], cstf, cstf[:, C_IOTA:C_IOTA + 128])
    identf = Tile(cstf[:, C_IDF:C_IDF + 128], cstf.keys)

    svt = P.static("svt", [128, 396], F32)
    stg = [P.static("stg%d" % i, [128, 1024], F32) for i in range(2)]
    cbf = [P.static("cbf%d" % i, [128, 1024], BF16) for i in range(2)]
    cnt = {"stg": 0, "cast": 0}

    def dx(tt):
        return P.dkey("x", tt)

    for tt in range(NT):
        P.dma("sp", "xcp", out_d.ap()[tt * 128:(tt + 1) * 128, :], x_d.ap()[tt * 128:(tt + 1) * 128, :], w=[dx(tt)])

    def load_cast(dst_t, dst_ap2, src_ap2, n):
        for c0 in range(0, n, 1024):
            w_ = min(1024, n - c0)
            i = cnt["stg"] % 2
            cnt["stg"] += 1
            P.dma("sp", "stg%d" % i, stg[i][:, 0:w_], src_ap2[:, c0:c0 + w_], w=[stg[i]])
            eng = ("dve", "act")[cnt["cast"] % 2]
            cnt["cast"] += 1
            CP(P, eng, dst_t, dst_ap2[:, c0:c0 + w_], stg[i], stg[i][:, 0:w_])

    def rstd_of(ss_t, ss_ap, n_inv, out_t, out_ap, tmp_t, tmp_ap):
        TS(P, "dve", tmp_t, tmp_ap, ss_t, ss_ap, n_inv, EPS, ALU.mult, ALU.add)
        ACTV(P, tmp_t, tmp_ap, tmp_t, tmp_ap, AF.Ln)
        ACTV(P, out_t, out_ap, tmp_t, tmp_ap, AF.Exp, scale=-0.5)

    def norm_transpose(xt, gt, xn, xnT_t, xnT_ap3, ss, tmp):
        ACTV(P, xn, xn[:, :], xt, xt[:, :], AF.Square, accum=ss[:, 0:1], w=[ss])
        rstd_of(ss, ss[:, 0:1], 1.0 / D, ss, ss[:, 1:2], tmp, tmp[:, 0:1])
        STT(P, xn, xn[:, :], xt, xt[:, :], ss[:, 1:2], gt, gt[:, :], ALU.mult, ALU.mult, r=[ss])
        for kc in range(8):
            TR(P, psT, psT[:, kc * 128:(kc + 1) * 128], xn, xn[:, kc * 128:(kc + 1) * 128], identb, identb[:, :])
        CP(P, "act", xnT_t, xnT_ap3, psT, psT[:, :].rearrange("p (a b) -> p a b", a=8))


    ffn_st = (P.static("v16", [128, 16, 16], F32), P.static("i16u", [128, 16, 16], U32), P.static("i16f", [128, 16, 16], F32),
              P.static("c16", [128, 8, 16], F32), P.static("p16u", [128, 8, 16], U32), P.static("p16f", [128, 8, 16], F32),
              P.static("kq", [128, 8, 16], F32), P.static("kr", [128, 8, 16], F32), P.static("isel", [128, 8, 16], F32),
              P.static("jsel", [128, 8, 16], F32), P.static("gsel", [128, 8, 16], F32), P.static("smf", [128, 32], F32),
              P.static("fss", [128, 4], F32))

    def ffn_layer(l):
        mb, mf = P.mark("abf"), P.mark("af")
        TB = 256 if S >= 256 else 128
        NTB = TB // 128
        wq_sb = P.alloc("abf", (8, 2048))
        keys_sb = P.alloc("abf", (16, 128))
        xn = P.alloc("abf", (D,))
        xnT = P.alloc("abf", (8, TB))
        qT = P.alloc("abf", (16, TB))
        selT = P.alloc("abf", (3, TB))
        OIg = P.alloc("abf", (16, 128))
        OJ = P.alloc("abf", (16, 128))
        W_sb = P.alloc("abf", (TB, 128))
        NR = 2
        u_r = [P.alloc("abf", (8, 128)) for _ in range(NR)]
        v_r = [P.alloc("abf", (D,)) for _ in range(NR)]
        Hh = [P.alloc("abf", (TB,)) for _ in range(2)]
        xres = [P.alloc("af", (D,)) for _ in range(NTB)]
        sc = P.alloc("af", (16, 128))
        sc2 = P.alloc("af", (16, 128))
        Gs = [P.alloc("af", (TB,)) for _ in range(2)]
        cand = Tile(sc.ap.rearrange("p a b -> p (a b)").rearrange("p (h x) -> p h x", h=8), sc.keys)
        cand2 = Tile(sc2.ap.rearrange("p a b -> p (a b)").rearrange("p (h x) -> p h x", h=8), sc2.keys)
        oh = Tile(sc2.ap.rearrange("p a b -> p (a b)").rearrange("p (h r k) -> p h r k", h=8, r=16), sc2.keys)
        (v16, i16u, i16f, c16, p16u, p16f, kq, kr, isel, jsel, gsel, sm, ss) = ffn_st
        g2t = P.alloc("af", (D,))

        P.dma("sp", "g2t", g2t[:, :], bcast_rows(gvec_d, (l * 3 + 1) * D, D), w=[g2t])
        load_cast(wq_sb, wq_sb.ap.rearrange("p a b -> p (a b)"), wq_d.ap()[l], 8 * 2048)
        load_cast(keys_sb, keys_sb.ap.rearrange("p a b -> p (a b)"), keys_d.ap()[l], 16 * 128)
        for j in range(128):
            for (src, dst, nm) in ((ut_d, ubf_d, "ubf"), (vt_d, vbf_d, "vbf")):
                i = cnt["stg"] % 2
                cnt["stg"] += 1
                P.dma("sp", "stg%d" % i, stg[i][:, 0:1024], src.ap()[l, j], w=[stg[i]])
                eng = ("dve", "act")[cnt["cast"] % 2]
                cnt["cast"] += 1
                CP(P, eng, cbf[i], cbf[i][:, :], stg[i], stg[i][:, 0:1024])
                P.dma("pool", "cbf%d" % i, dst.ap()[l, j], cbf[i][:, :], r=[cbf[i]], w=[P.dkey(nm, l, j)])

        iota3 = AP(iotab.ap.tensor, iotab.ap.offset, [list(iotab.ap.ap[0]), [0, 16], [1, 128]])
        iota16_4 = AP(cstf.ap.tensor, cstf.ap.offset + C_IOTA16, [list(cstf.ap.ap[0]), [0, 8], [0, 16], [1, 16]])
        thr_4 = AP(cstf.ap.tensor, cstf.ap.offset + C_THR, [list(cstf.ap.ap[0]), [0, 8], [0, 16], [1, 16]])
        misc = [ps[6], psTf]
        mcnt = [0]

        def mbank():
            b = misc[mcnt[0] % 2]
            mcnt[0] += 1
            return b

        for blk in range(S // TB):
            for ti in range(NTB):
                tt = blk * NTB + ti
                P.dma("sp", "xres%d" % ti, xres[ti][:, :], out_d.ap()[tt * 128:(tt + 1) * 128, :], r=[dx(tt)], w=[xres[ti]])
                norm_transpose(xres[ti], g2t, xn, xnT, xnT[:, :, ti * 128:(ti + 1) * 128], ss, sm)
            npb = 512 // TB
            for h0 in range(0, 16, npb):
                b = mbank()
                for s_ in range(npb):
                    hc = h0 + s_
                    for kc in range(8):
                        MM(P, b, b[:, s_ * TB:(s_ + 1) * TB], wq_sb, wq_sb[:, kc, hc * 128:(hc + 1) * 128],
                           xnT, xnT[:, kc, :], start=(kc == 0), stop=(kc == 7))
                CP(P, "act", qT, qT[:, h0:h0 + npb, :], b, b[:, :].rearrange("p (a b) -> p a b", a=npb))
            for ti in range(NTB):
                tsl = slice(ti * 128, (ti + 1) * 128)
                for q4 in range(4):
                    b = mbank()
                    for s4 in range(4):
                        hc = 4 * q4 + s4
                        MM(P, b, b[:, s4 * 128:(s4 + 1) * 128], qT, qT[:, hc, tsl], keys_sb, keys_sb[:, hc, :])
                    CP(P, "act", sc, sc[:, 4 * q4:4 * q4 + 4, :], b, b[:, :].rearrange("p (a b) -> p a b", a=4))
                for hc in range(16):
                    P.dve(lambda e, hc=hc: e.max(out=v16[:, hc, 0:8], in_=sc[:, hc, :]), r=[sc], w=[v16])
                    P.dve(lambda e, hc=hc: e.max_index(out=i16u[:, hc, 0:8], in_max=v16[:, hc, 0:8], in_values=sc[:, hc, :]),
                          r=[sc, v16], w=[i16u])
                    P.dve(lambda e, hc=hc: e.match_replace(out=sc2[:, hc, :], in_to_replace=v16[:, hc, 0:8],
                                                           in_values=sc[:, hc, :], imm_value=NEG), r=[sc, v16], w=[sc2])
                    P.dve(lambda e, hc=hc: e.max(out=v16[:, hc, 8:16], in_=sc2[:, hc, :]), r=[sc2], w=[v16])
                    P.dve(lambda e, hc=hc: e.max_index(out=i16u[:, hc, 8:16], in_max=v16[:, hc, 8:16], in_values=sc2[:, hc, :]),
                          r=[sc2, v16], w=[i16u])
                CP(P, "dve", i16f, i16f[:, :, :], i16u, i16u[:, :, :])
                v4 = v16.ap.rearrange("p (h c) k -> p h c k", c=2)
                i4 = i16f.ap.rearrange("p (h c) k -> p h c k", c=2)
                TT(P, "dve", cand, cand.ap.rearrange("p h (a b) -> p h a b", a=16), v16, bc(v4[:, :, 0, :], 3, 16),
                   v16, bc(v4[:, :, 1, :], 2, 16), ALU.add)
                for h in range(8):
                    P.dve(lambda e, h=h: e.max(out=c16[:, h, 0:8], in_=cand[:, h, :]), r=[cand], w=[c16])
                    P.dve(lambda e, h=h: e.max_index(out=p16u[:, h, 0:8], in_max=c16[:, h, 0:8], in_values=cand[:, h, :]),
                          r=[cand, c16], w=[p16u])
                    P.dve(lambda e, h=h: e.match_replace(out=cand2[:, h, :], in_to_replace=c16[:, h, 0:8],
                                                         in_values=cand[:, h, :], imm_value=NEG), r=[cand, c16], w=[cand2])
                    P.dve(lambda e, h=h: e.max(out=c16[:, h, 8:16], in_=cand2[:, h, :]), r=[cand2], w=[c16])
                    P.dve(lambda e, h=h: e.max_index(out=p16u[:, h, 8:16], in_max=c16[:, h, 8:16], in_values=cand2[:, h, :]),
                          r=[cand2, c16], w=[p16u])
                CP(P, "dve", p16f, p16f[:, :, :], p16u, p16u[:, :, :])
                TT(P, "dve", oh, oh[:, :, :, :], p16f, bc(p16f[:, :, :], 3, 16), cstf, thr_4, ALU.is_ge)
                RED(P, kq, kq[:, :, :], oh, oh[:, :, :, :], ALU.add)
                STT(P, kr, kr[:, :, :], kq, kq[:, :, :], -16.0, p16f, p16f[:, :, :], ALU.mult, ALU.add)
                for (kk, src, dst) in ((kq, i4[:, :, 0, :], isel), (kr, i4[:, :, 1, :], jsel)):
                    TT(P, "dve", oh, oh[:, :, :, :], kk, bc(kk[:, :, :], 3, 16), cstf, iota16_4, ALU.is_equal)
                    TT(P, "dve", oh, oh[:, :, :, :], oh, oh[:, :, :, :], i16f, bc(src, 2, 16), ALU.mult)
                    RED(P, dst, dst[:, :, :], oh, oh[:, :, :, :], ALU.add)
                RED(P, sm, sm[:, 0:8], c16, c16[:, :, :], ALU.max)
                TT(P, "dve", gsel, gsel[:, :, :], c16, c16[:, :, :], sm, bc(sm[:, 0:8], 2, 16), ALU.subtract)
                ACTV(P, gsel, gsel[:, :, :], gsel, gsel[:, :, :], AF.Exp)
                RED(P, sm, sm[:, 8:16], gsel, gsel[:, :, :], ALU.add)
                P.dve(lambda e: e.reciprocal(out=sm[:, 16:24], in_=sm[:, 8:16]), r=[sm], w=[sm])
                TT(P, "dve", gsel, gsel[:, :, :], gsel, gsel[:, :, :], sm, bc(sm[:, 16:24], 2, 16), ALU.mult)
                b = mbank()
                for q_, src in enumerate((isel, jsel, gsel)):
                    TR(P, b, b[:, q_ * 128:(q_ + 1) * 128], src, src.ap.rearrange("p h r -> p (h r)"), identf, identf[:, :])
                CP(P, "act", selT, selT[:, :, tsl], b, b[:, 0:384].rearrange("p (a b) -> p a b", a=3))
            for tb in range(TB // 16):
                t0 = tb * 16
                TT(P, "dve", OIg, OIg[:, :, :], iotab, iota3, selT, bc(selT[:, 0, t0:t0 + 16], 2, 128), ALU.is_equal)
                TT(P, "dve", OIg, OIg[:, :, :], OIg, OIg[:, :, :], selT, bc(selT[:, 2, t0:t0 + 16], 2, 128), ALU.mult)
                TT(P, "dve", OJ, OJ[:, :, :], iotab, iota3, selT, bc(selT[:, 1, t0:t0 + 16], 2, 128), ALU.is_equal)
                for q_ in range(4):
                    b = mbank()
                    for u_ in range(4):
                        t = q_ * 4 + u_
                        MM(P, b, b[:, u_ * 128:(u_ + 1) * 128], OIg, OIg[:, t, :], OJ, OJ[:, t, :])
                    CP(P, ("act", "dve")[q_ % 2], W_sb, W_sb[:, t0 + q_ * 4:t0 + q_ * 4 + 4, :], b,
                       b[:, :].rearrange("p (a b) -> p a b", a=4))
            for j in range(128):
                sl = j % NR
                P.dma("sp", "u%d" % sl, u_r[sl].ap.rearrange("p a b -> p (a b)"), ubf_d.ap()[l, j],
                      r=[P.dkey("ubf", l, j)], w=[u_r[sl]])
                P.dma("sp", "v%d" % sl, v_r[sl][:, :], vbf_d.ap()[l, j], r=[P.dkey("vbf", l, j)], w=[v_r[sl]])
                ab = ps[4 + (j % 2)]
                for kc in range(8):
                    MM(P, ab, ab[:, 0:TB], u_r[sl], u_r[sl][:, kc, :], xnT, xnT[:, kc, :], start=(kc == 0), stop=(kc == 7))
                g_ = Gs[j % 2]
                h_ = Hh[j % 2]
                ACTV(P, g_, g_[:, :], ab, ab[:, 0:TB], AF.Gelu_apprx_tanh)
                TT(P, "dve", h_, h_[:, :], g_, g_[:, :], W_sb, W_sb[:, :, j], ALU.mult)
                for ti in range(NTB):
                    for hf in range(2):
                        yb = ps[ti * 2 + hf]
                        MM(P, yb, yb[:, :], h_, h_[:, ti * 128:(ti + 1) * 128], v_r[sl], v_r[sl][:, hf * 512:(hf + 1) * 512],
                           start=(j == 0), stop=(j == 127))
            for ti in range(NTB):
                tt = blk * NTB + ti
                for hf in range(2):
                    yb = ps[ti * 2 + hf]
                    TT(P, "dve", xres[ti], xres[ti][:, hf * 512:(hf + 1) * 512], xres[ti], xres[ti][:, hf * 512:(hf + 1) * 512],
                       yb, yb[:, :], ALU.add)
                P.dma("pool", "stx%d" % ti, out_d.ap()[tt * 128:(tt + 1) * 128, :], xres[ti][:, :], r=[xres[ti]], w=[dx(tt)])
        P.reset("abf", mb)
        P.reset("af", mf)


    QG = min(512, S)
    TG = QG // 128
    qT_d = nc.dram_tensor("qTd", [NT, 128, 384], BF16, kind="Internal")
    trib = P.static("trib", [128, 128], BF16)
    ntrib = P.static("ntrib", [128, 128], BF16)
    noneb = P.static("noneb", [128, 128], BF16)
    CP(P, "dve", trib, trib[:, :], cstf, cstf[:, C_TRI:C_TRI + 128])
    CP(P, "dve", ntrib, ntrib[:, :], cstf, cstf[:, C_NTRI:C_NTRI + 128])
    CP(P, "dve", noneb, noneb[:, :], cstf, cstf[:, C_NONE:C_NONE + 128])
    trif = Tile(cstf[:, C_TRI:C_TRI + 128], cstf.keys)
    cw = P.static("cw", [128, 6, 5], F32)
    sgb = P.static("sgb", [128, 4], F32)
    Cn = P.static("Cn", [128, 3, 65], F32)
    Cb = P.static("Cb", [128, 3, 65], BF16)
    cvin = P.static("cvin", [128, 6, 131], F32)
    v1 = P.static("v1", [128, 6, 65], BF16)
    sm2 = P.static("sm2", [128, 64], F32)
    ss2 = P.static("ss2", [128, 4], F32)
    gif = P.static("gif", [128, 12], F32)
    nlf = P.static("nlf", [128, 6], F32)
    nb = P.static("nb", [128, 6], F32)
    nhl = P.static("nhl", [128, 2, 6], BF16)
    nh32 = P.static("nh32", [128, 6], F32)
    nbh = P.static("nbh", [128, 2, 6, 128], BF16)
    nbp = P.static("nbp", [128, 2, 3, 128], BF16)
    bD = P.static("bD", [128, 6], F32)
    wsv = P.static("wsv", [128, 6], F32)
    dec = P.static("dec", [128, 3], F32)
    one_t = P.static("one_t", [128, 1], F32)
    P.dve(lambda e: e.memset(one_t[:, :], 1.0), w=[one_t])

    def mix_layer(l):
        mb, mf = P.mark("abf"), P.mark("af")
        wout = P.alloc("abf", (8, 1024))
        kTa = P.alloc("abf", (3, S))
        va = P.alloc("abf", (NT, 384))
        g1t_ = P.alloc("af", (D,))
        ogt_ = P.alloc("af", (D,))
        xt = P.alloc("af", (D,))
        mb1, mf1 = P.mark("abf"), P.mark("af")
        winA = P.alloc("abf", (8, COLS_A))
        winB = P.alloc("abf", (8, 768))
        sgw = P.alloc("abf", (4, 128))
        xn = P.alloc("abf", (D,))
        xnT = P.alloc("abf", (8, 128))
        qkb = P.alloc("abf", (768,))
        mqT = P.alloc("abf", (3, 128))
        mkT = P.alloc("abf", (3, 128))
        qsTz = P.alloc("abf", (6, 128))
        ebp = P.alloc("abf", (3, 128))
        mktok = P.alloc("abf", (384,))
        kw = P.alloc("abf", (384,))
        PT = P.alloc("abf", (6, 128))
        mqTz = Tile(PT.ap, PT.keys)
        og = P.alloc("abf", (384,))
        ymsb = P.alloc("abf", (640,))
        sgvb = P.alloc("abf", (256,))
        qst = P.alloc("abf", (384,))
        zq = P.alloc("af", (768,))
        uv = P.alloc("af", (512,))
        cva = P.alloc("af", (6, 128))
        yb = P.alloc("af", (D,))
        DTf = Tile(cva.ap, cva.keys)

        P.dma("sp", "g1t", g1t_[:, :], bcast_rows(gvec_d, (l * 3 + 0) * D, D), w=[g1t_])
        P.dma("sp", "ogt", ogt_[:, :], bcast_rows(gvec_d, (l * 3 + 2) * D, D), w=[ogt_])
        P.dma("sp", "svt", svt[:, :], bcast_rows(svec_d, l * 396, 396), w=[svt])
        P.dma("sp", "cw", cw.ap.rearrange("p a b -> p (a b)"), convw_d.ap()[l], w=[cw])
        P.dma("sp", "sgb", sgb[:, :], sgb_d.ap()[l], w=[sgb])
        load_cast(winA, winA.ap.rearrange("p a b -> p (a b)"), wina_d.ap()[l], 8 * COLS_A)
        load_cast(winB, winB.ap.rearrange("p a b -> p (a b)"), winb_d.ap()[l], 8 * 768)
        load_cast(wout, wout.ap.rearrange("p a b -> p (a b)"), wout_d.ap()[l], 8 * 1024)
        P.dma("sp", "stg0", stg[0][:, 0:512], sgw_d.ap()[l], w=[stg[0]])
        TT(P, "dve", sgw, sgw[:, :, :], stg[0], stg[0][:, 0:512].rearrange("p (g t) -> p g t", g=4), cstf,
           bc(cstf[:, C_TRI:C_TRI + 128], 1, 4), ALU.mult)
        P.dve(lambda e: e.memset(Cn[:, :, :], 0.0), w=[Cn])
        P.dve(lambda e: e.memset(Cb[:, :, :], 0.0), w=[Cb])
        P.dve(lambda e: e.memset(cvin[:, :, 0:3], 0.0), w=[cvin])
        P.dve(lambda e: e.memset(v1[:, :, 64:65], 1.0), w=[v1])

        chunks = [(0, 512), (512, 512), (1024, 512), (1536, 512), (2048, 396)]
        for tt in range(NT):
            tsl = slice(tt * 128, (tt + 1) * 128)
            P.dma("sp", "xt", xt[:, :], out_d.ap()[tsl, :], r=[dx(tt)], w=[xt])
            norm_transpose(xt, g1t_, xn, xnT, xnT[:, :, :], ss2, sm2)
            for ci, (c0, cw_) in enumerate(chunks):
                b = ps[ci % 2]
                for kc in range(8):
                    MM(P, b, b[:, 0:cw_], xnT, xnT[:, kc, :], winA, winA[:, kc, c0:c0 + cw_], start=(kc == 0), stop=(kc == 7))
                if ci == 0:
                    CP(P, "act", zq, zq[:, 0:512], b, b[:, 0:512])
                elif ci == 1:
                    CP(P, "act", zq, zq[:, 512:768], b, b[:, 0:256])
                    CP(P, "act", va, va[:, tt, 0:256], b, b[:, 256:512])
                elif ci == 2:
                    CP(P, "act", va, va[:, tt, 256:384], b, b[:, 0:128])
                    CP(P, "act", v1, v1[:, :, 0:64], b, b[:, 128:512].rearrange("p (h d) -> p h d", h=6))
                elif ci == 3:
                    ACTV(P, og, og[:, :], b, b[:, 0:384], AF.Sigmoid)
                    ACTV(P, uv, uv[:, 0:128], b, b[:, 384:512], AF.Gelu_apprx_tanh)
                else:
                    ACTV(P, uv, uv[:, 128:512], b, b[:, 0:384], AF.Gelu_apprx_tanh)
                    TT(P, "dve", gif, gif[:, :], b, b[:, 384:396], svt, svt[:, 384:396], ALU.add)
            P.pos("zT")
            for c in range(6):
                b = ps[2] if c < 4 else ps[3]
                o_ = (c % 4) * 128
                for kc in range(8):
                    MM(P, b, b[:, o_:o_ + 128], winB, winB[:, kc, c * 128:(c + 1) * 128], xnT, xnT[:, kc, :],
                       start=(kc == 0), stop=(kc == 7))
            CP(P, "act", cvin, cvin[:, 0:4, 3:131], ps[2], ps[2][:, :].rearrange("p (a b) -> p a b", a=4))
            CP(P, "act", cvin, cvin[:, 4:6, 3:131], ps[3], ps[3][:, 0:256].rearrange("p (a b) -> p a b", a=2))
            P.pos("sbnorm")
            z12 = zq.ap.rearrange("p (h d) -> p h d", h=12)
            y12 = yb.ap[:, 0:768].rearrange("p (h d) -> p h d", h=12)
            TT(P, "dve", yb, y12, zq, z12, zq, z12, ALU.mult)
            RED(P, sm2, sm2[:, 0:12], yb, y12, ALU.add)
            rstd_of(sm2, sm2[:, 0:12], 1.0 / 64, sm2, sm2[:, 16:28], sm2, sm2[:, 32:44])
            TS(P, "dve", sm2, sm2[:, 16:22], sm2, sm2[:, 16:22], 0.125, None, ALU.mult)
            TT(P, "dve", zq, z12, zq, z12, sm2, bc(sm2[:, 16:28], 2, 64), ALU.mult)
            gqk = AP(svt.ap.tensor, svt.ap.offset, [list(svt.ap.ap[0]), [64, 2], [0, 6], [1, 64]])
            TT(P, "dve", qkb, qkb.ap.rearrange("p (c h d) -> p c h d", c=2, h=6), zq,
               zq.ap.rearrange("p (c h d) -> p c h d", c=2, h=6), svt, gqk, ALU.mult)
            for p_ in range(6):
                TR(P, psT, psT[:, p_ * 128:(p_ + 1) * 128], qkb, qkb[:, p_ * 128:(p_ + 1) * 128], identb, identb[:, :])
            CP(P, "act", qst, qst[:, :], psT, psT[:, 0:384])
            P.dma("pool", "qst", qT_d.ap()[tt], qst[:, :], r=[qst], w=[P.dkey("qT", tt)])
            CP(P, "act", kTa, kTa[:, :, tsl], psT, psT[:, 384:768].rearrange("p (a b) -> p a b", a=3))
            P.pos("conv")
            y6 = yb.ap[:, 0:768].rearrange("p (c t) -> p c t", c=6)
            TT(P, "dve", cva, cva[:, :, :], cvin, cvin[:, :, 0:128], cw, bc(cw[:, :, 0], 2, 128), ALU.mult)
            TT(P, "dve", cva, cva[:, :, :], cva, cva[:, :, :], cw, bc(cw[:, :, 4], 2, 128), ALU.add)
            for j in range(1, 4):
                TT(P, "dve", yb, y6, cvin, cvin[:, :, j:j + 128], cw, bc(cw[:, :, j], 2, 128), ALU.mult)
                TT(P, "dve", cva, cva[:, :, :], cva, cva[:, :, :], yb, y6, ALU.add)
            CP(P, "dve", cvin, cvin[:, :, 0:3], cvin, cvin[:, :, 128:131])
            ACTV(P, yb, y6, cva, cva[:, :, :], AF.Sigmoid)
            TT(P, "dve", mqT, mqT[:, :, :], cva, cva[:, 0:3, :], yb, y6[:, 0:3, :], ALU.mult)
            STT(P, mkT, mkT[:, :, :], cva, cva[:, 3:6, :], 0.125, yb, y6[:, 3:6, :], ALU.mult, ALU.mult)
            for p_ in range(3):
                TR(P, psT, psT[:, p_ * 128:(p_ + 1) * 128], mkT, mkT[:, p_, :], identb, identb[:, :])
            CP(P, "act", mktok, mktok[:, :], psT, psT[:, 0:384])
            P.pos("gates")
            ACTV(P, nlf, nlf[:, :], gif, gif[:, 6:12], AF.Exp, scale=-1.0)
            ACTV(P, nlf, nlf[:, :], nlf, nlf[:, :], AF.Ln, bias=one_t[:, 0:1], r=[one_t])
            b6 = ps[6]
            CP(P, "dve", nhl, nhl[:, 0, :], nlf, nlf[:, :])
            CP(P, "dve", nh32, nh32[:, :], nhl, nhl[:, 0, :])
            TT(P, "dve", nhl, nhl[:, 1, :], nlf, nlf[:, :], nh32, nh32[:, :], ALU.subtract)
            CP(P, "dve", nbh, nbh[:, :, :, :], nhl, bc(nhl[:, :, :], 3, 128))
            CP(P, "dve", nbp, nbp.ap.rearrange("p c a (b d) -> p (c a) b d", b=2), nhl,
               bc(nhl.ap.rearrange("p c (a b) -> p (c a) b", a=3), 3, 64))
            for h in range(6):
                b = ps[4] if h < 4 else ps[5]
                o_ = (h % 4) * 128
                for c_ in range(2):
                    MM(P, b, b[:, o_:o_ + 128], nbh, nbh[:, c_, h, :], trib, trib[:, :], start=(c_ == 0), stop=(c_ == 1))
            for p_ in range(3):
                for c_ in range(2):
                    MM(P, b6, b6[:, p_ * 128:(p_ + 1) * 128], nbp, nbp[:, c_, p_, :], trib, trib[:, :], start=(c_ == 0), stop=(c_ == 1))
            yd = yb.ap[:, 0:768].rearrange("p (h t) -> p h t", h=6)
            TT(P, "dve", yb, yd[:, 0:4, :], ps[4], ps[4][:, :].rearrange("p (a b) -> p a b", a=4), cstf, bc(cstf[:, C_IDF:C_IDF + 128], 1, 4), ALU.mult)
            TT(P, "dve", yb, yd[:, 4:6, :], ps[5], ps[5][:, 0:256].rearrange("p (a b) -> p a b", a=2), cstf, bc(cstf[:, C_IDF:C_IDF + 128], 1, 2), ALU.mult)
            RED(P, nb, nb[:, :], yb, yd, ALU.add)
            TT(P, "dve", bD, bD[:, :], nb, nb[:, :], gif, gif[:, 0:6], ALU.add)
            for h in range(6):
                b = ps[4] if h < 4 else ps[5]
                o_ = (h % 4) * 128
                ACTV(P, DTf, DTf[:, h, :], b, b[:, o_:o_ + 128], AF.Exp, bias=bD[:, h:h + 1], scale=-1.0, r=[bD])
            TT(P, "dve", DTf, DTf[:, :, :], DTf, DTf[:, :, :], cstf, bc(cstf[:, C_TRI:C_TRI + 128], 1, 6), ALU.mult)
            ACTV(P, ebp, ebp[:, :, :], b6, b6[:, 0:384].rearrange("p (a b) -> p a b", a=3), AF.Exp, scale=-1.0)
            ACTV(P, dec, dec[:, :], b6, b6[:, 0:384].rearrange("p (a b) -> p a b", a=3)[:, :, 127], AF.Exp, scale=-1.0)
            TT(P, "dve", wsv, wsv[:, 0:4], bD, bD[:, 0:4], ps[4], ps[4][:, :].rearrange("p (a b) -> p a b", a=4)[:, :, 127],
               ALU.subtract)
            TT(P, "dve", wsv, wsv[:, 4:6], bD, bD[:, 4:6], ps[5], ps[5][:, 0:256].rearrange("p (a b) -> p a b", a=2)[:, :, 127],
               ALU.subtract)
            ACTV(P, wsv, wsv[:, :], wsv, wsv[:, :], AF.Exp)
            P.dve(lambda e: e.memset(mqTz[:, :, :], 0.0), w=[mqTz])
            mz4 = mqTz.ap.rearrange("p (a b) t -> p a b t", b=2)
            CP(P, "dve", mqTz, mz4[0:64, :, 0, :], mqT, mqT[0:64, :, :])
            CP(P, "dve", mqTz, mz4[64:128, :, 1, :], mqT, mqT[64:128, :, :])
            TT(P, "dve", qsTz, qsTz.ap.rearrange("p (a b) t -> p a b t", b=2), mqTz, mz4, ebp, bc(ebp[:, :, :], 2, 2), ALU.mult)
            TT(P, "dve", kw, kw.ap.rearrange("p (h d) -> p h d", h=6), mktok, mktok.ap.rearrange("p (h d) -> p h d", h=6),
               wsv, bc(wsv[:, :], 2, 64), ALU.mult)
            P.pos("ST")
            for h in range(6):
                b = ps[2] if h < 4 else ps[3]
                o_ = (h % 4) * 128
                p_, r0 = h // 2, (h % 2) * 64
                MM(P, b, b[:, o_:o_ + 128], mkT, mkT[:, p_, :], mqTz, mqTz[:, h, :])
            TT(P, "dve", PT, PT[:, 0:4, :], ps[2], ps[2][:, :].rearrange("p (a b) -> p a b", a=4), DTf, DTf[:, 0:4, :], ALU.mult)
            TT(P, "dve", PT, PT[:, 4:6, :], ps[3], ps[3][:, 0:256].rearrange("p (a b) -> p a b", a=2), DTf, DTf[:, 4:6, :], ALU.mult)
            P.pos("numden")
            nd = ps[0]
            for h in range(6):
                p_, r0 = h // 2, (h % 2) * 64
                MM(P, nd, nd[:, h * 65:(h + 1) * 65], PT, PT[:, h, :], v1, v1[:, h, :], start=True, stop=(tt == 0))
                if tt > 0:
                    MM(P, nd, nd[:, h * 65:(h + 1) * 65], qsTz, qsTz[:, h, :], Cb, Cb[:, p_, :], start=False, stop=True)
            nd3 = nd[:, 0:390].rearrange("p (h e) -> p h e", h=6)
            CP(P, "dve", sm2, sm2[:, 40:46], nd, nd3[:, :, 64])
            TS(P, "dve", sm2, sm2[:, 48:54], sm2, sm2[:, 40:46], -1.0, None, ALU.mult)
            TT(P, "dve", sm2, sm2[:, 48:54], sm2, sm2[:, 48:54], sm2, sm2[:, 40:46], ALU.max)
            TS(P, "dve", sm2, sm2[:, 48:54], sm2, sm2[:, 48:54], 1.0, None, ALU.max)
            P.dve(lambda e: e.reciprocal(out=sm2[:, 56:62], in_=sm2[:, 48:54]), r=[sm2], w=[sm2])
            TT(P, "dve", yb, yb[:, 384:768].rearrange("p (h d) -> p h d", h=6), nd, nd3[:, :, 0:64], sm2, bc(sm2[:, 56:62], 2, 64),
               ALU.mult)
            P.pos("state")
            su = ps[1]
            for p_ in range(3):
                for s_ in range(2):
                    h = 2 * p_ + s_
                    MM(P, su, su[:, h * 65:(h + 1) * 65], kw, kw[:, p_ * 128:(p_ + 1) * 128], v1, v1[:, h, :])
            for p_ in range(3):
                for s_ in range(2):
                    h = 2 * p_ + s_
                    r0 = s_ * 64
                    STT(P, Cn, Cn[r0:r0 + 64, p_, :], Cn, Cn[r0:r0 + 64, p_, :], dec[r0:r0 + 64, p_:p_ + 1], su,
                        su[r0:r0 + 64, h * 65:(h + 1) * 65], ALU.mult, ALU.add, r=[dec])
            CP(P, "dve", Cb, Cb[:, :, :], Cn, Cn[:, :, :])
            P.pos("sg")
            u4 = uv.ap[:, 0:256].rearrange("p (g d) -> p g d", g=4)
            v4_ = uv.ap[:, 256:512].rearrange("p (g d) -> p g d", g=4)
            z4 = zq.ap[:, 0:256].rearrange("p (g d) -> p g d", g=4)
            TT(P, "dve", zq, z4, uv, v4_, uv, v4_, ALU.mult)
            RED(P, sm2, sm2[:, 0:4], zq, z4, ALU.add)
            rstd_of(sm2, sm2[:, 0:4], 1.0 / 64, sm2, sm2[:, 16:20], sm2, sm2[:, 32:36])
            TT(P, "dve", uv, v4_, uv, v4_, sm2, bc(sm2[:, 16:20], 2, 64), ALU.mult)
            TT(P, "dve", sgvb, sgvb[:, :], uv, uv[:, 256:512], svt, svt[:, 128:384], ALU.mult)
            gb = ps[5]
            for g in range(4):
                MM(P, gb, gb[:, 256 + g * 64:256 + (g + 1) * 64], sgw, sgw[:, g, :], sgvb, sgvb[:, g * 64:(g + 1) * 64])
            y4 = yb.ap[:, 768:1024].rearrange("p (g d) -> p g d", g=4)
            TT(P, "dve", yb, y4, gb, gb[:, 256:512].rearrange("p (g d) -> p g d", g=4), sgb, bc(sgb[:, :], 2, 64), ALU.add)
            TT(P, "dve", yb, y4, yb, y4, uv, u4, ALU.mult)
            P.pos("onorm")
            y10 = yb.ap[:, 384:1024].rearrange("p (h d) -> p h d", h=10)
            z10 = zq.ap[:, 0:640].rearrange("p (h d) -> p h d", h=10)
            TT(P, "dve", zq, z10, yb, y10, yb, y10, ALU.mult)
            RED(P, sm2, sm2[:, 0:10], zq, z10, ALU.add)
            rstd_of(sm2, sm2[:, 0:10], 1.0 / 64, sm2, sm2[:, 16:26], sm2, sm2[:, 32:42])
            TT(P, "dve", yb, y10, yb, y10, sm2, bc(sm2[:, 16:26], 2, 64), ALU.mult)
            TT(P, "dve", yb, yb[:, 384:1024], yb, yb[:, 384:1024], ogt_, ogt_[:, 384:1024], ALU.mult)
            TT(P, "dve", ymsb, ymsb[:, 0:384], yb, yb[:, 384:768], og, og[:, :], ALU.mult)
            CP(P, "act", ymsb, ymsb[:, 384:640], yb, yb[:, 768:1024])
            P.dma("pool", "ymsb", yms_d.ap()[tsl, :], ymsb[:, :], r=[ymsb], w=[P.dkey("yms", tt)])
        P.reset("abf", mb1)
        P.reset("af", mf1)
        P.pos("M2")
        mask4b = P.alloc("abf", (4, 512))
        y_bf = P.alloc("abf", (D,))
        yT = P.alloc("abf", (8, 128))
        qTg = P.alloc("abf", (3, QG))
        qTgz = P.alloc("abf", (6, QG))
        spb = [P.alloc("abf", (QG,)) for _ in range(2)]
        Lacc = [P.alloc("abf", (QG,)) for _ in range(2)]
        wT = [P.alloc("abf", (QG,)) for _ in range(2)]
        e32 = [P.alloc("af", (QG,)) for _ in range(2)]
        ysb = P.alloc("af", (TG, 384))
        sqb = P.alloc("af", (384,))
        yfs = P.alloc("af", (QG,))
        for r_ in range(4):
            P.dma("sp", "stg1", stg[1][:, 0:512], cst_d.ap()[:, C_MASK4 + r_ * 512:C_MASK4 + (r_ + 1) * 512], w=[stg[1]])
            CP(P, "dve", mask4b, mask4b[:, r_, :], stg[1], stg[1][:, 0:512])
        for g in range(S // QG):
            for i in range(TG):
                P.dma("sp", "qTg", qTg[:, :, i * 128:(i + 1) * 128], qT_d.ap()[g * TG + i].rearrange("p (a b) -> p a b", a=3),
                      r=[P.dkey("qT", g * TG + i)], w=[qTg])
            nkb = (g + 1) * TG
            P.dve(lambda e: e.memset(qTgz[:, :, :], 0.0), w=[qTgz])
            qz4 = qTgz.ap.rearrange("p (a b) t -> p a b t", b=2)
            CP(P, "dve", qTgz, qz4[0:64, :, 0, :], qTg, qTg[0:64, :, :])
            CP(P, "dve", qTgz, qz4[64:128, :, 1, :], qTg, qTg[64:128, :, :])
            for h in range(6):
                p_, r0 = h // 2, (h % 2) * 64
                ya = ps[4 + h % 2]
                it = 0
                for kb in range(nkb - 1, -1, -1):
                    rr = kb - g * TG
                    first = (kb == nkb - 1)
                    zb = ps[it % 2]
                    Bb = ps[2 + it % 2]
                    kap = kTa[:, p_, kb * 128:(kb + 1) * 128]
                    qap = qTgz[:, h, :]
                    MM(P, zb, zb[:, 0:QG], kTa, kap, qTgz, qap)
                    e_ = e32[it % 2]
                    s_ = spb[it % 2]
                    w_ = wT[it % 2]
                    ACTV(P, e_, e_[:, :], zb, zb[:, 0:QG], AF.Exp)
                    ACTV(P, s_, s_[:, :], e_, e_[:, :], AF.Ln, bias=one_t[:, 0:1], r=[one_t])
                    if rr >= 0:
                        TT(P, "dve", s_, s_[:, :], s_, s_[:, :], mask4b, mask4b[:, rr, 0:QG], ALU.mult)
                    MM(P, Bb, Bb[:, 0:QG], kTa, kap, qTgz, qap, start=True, stop=False)
                    MM(P, Bb, Bb[:, 0:QG], ntrib, ntrib[:, :], s_, s_[:, :], start=False, stop=first)
                    if not first:
                        MM(P, Bb, Bb[:, 0:QG], noneb, noneb[:, :], Lacc[it % 2], Lacc[it % 2][:, :], start=False, stop=True)
                    ACTV(P, w_, w_[:, :], Bb, Bb[:, 0:QG], AF.Exp)
                    if rr >= 0:
                        TT(P, "dve", w_, w_[:, :], w_, w_[:, :], mask4b, mask4b[:, rr, 0:QG], ALU.mult)
                    if kb > 0:
                        if first:
                            CP(P, "dve", Lacc[(it + 1) % 2], Lacc[(it + 1) % 2][:, :], s_, s_[:, :])
                        else:
                            TT(P, "dve", Lacc[(it + 1) % 2], Lacc[(it + 1) % 2][:, :], Lacc[it % 2], Lacc[it % 2][:, :], s_, s_[:, :],
                               ALU.add)
                    MM(P, ya, ya[:, 0:QG], va, va[:, kb, p_ * 128:(p_ + 1) * 128], w_, w_[:, :], start=first, stop=(kb == 0))
                    it += 1
                CP(P, "act", yfs, yfs[:, :], ya, ya[:, 0:QG])
                tb_ = ps[6]
                for qt in range(TG):
                    TR(P, tb_, tb_[:, qt * 128:(qt + 1) * 128], yfs, yfs[:, qt * 128:(qt + 1) * 128], identf, identf[:, :])
                CP(P, "act", ysb, ysb.ap.rearrange("p t (h d) -> p t h d", h=6)[:, :, h, :], tb_,
                   tb_[:, 0:TG * 128].rearrange("p (t d) -> p t d", t=TG)[:, :, r0:r0 + 64])
            for qt in range(TG):
                tt = g * TG + qt
                tsl = slice(tt * 128, (tt + 1) * 128)
                y6_ = ysb.ap[:, qt, :].rearrange("p (h d) -> p h d", h=6)
                s6_ = sqb.ap.rearrange("p (h d) -> p h d", h=6)
                TT(P, "dve", sqb, s6_, ysb, y6_, ysb, y6_, ALU.mult)
                RED(P, sm2, sm2[:, 0:6], sqb, s6_, ALU.add)
                rstd_of(sm2, sm2[:, 0:6], 1.0 / 64, sm2, sm2[:, 16:22], sm2, sm2[:, 32:38])
                TT(P, "dve", sqb, s6_, ysb, y6_, sm2, bc(sm2[:, 16:22], 2, 64), ALU.mult)
                TT(P, "dve", y_bf, y_bf[:, 0:384], sqb, sqb[:, :], ogt_, ogt_[:, 0:384], ALU.mult)
                P.dma("sp", "ybf", y_bf[:, 384:1024], yms_d.ap()[tsl, :], r=[P.dkey("yms", tt)], w=[y_bf])
                for kc in range(8):
                    TR(P, psT, psT[:, kc * 128:(kc + 1) * 128], y_bf, y_bf[:, kc * 128:(kc + 1) * 128], identb, identb[:, :])
                CP(P, "act", yT, yT[:, :, :], psT, psT[:, :].rearrange("p (a b) -> p a b", a=8))
                P.dma("sp", "xt", xt[:, :], out_d.ap()[tsl, :], r=[dx(tt)], w=[xt])
                for hf in range(2):
                    ob = ps[6] if hf == 0 else ps[0]
                    for kc in range(8):
                        MM(P, ob, ob[:, :], yT, yT[:, kc, :], wout, wout[:, kc, hf * 512:(hf + 1) * 512], start=(kc == 0), stop=(kc == 7))
                    TT(P, "dve", xt, xt[:, hf * 512:(hf + 1) * 512], xt, xt[:, hf * 512:(hf + 1) * 512], ob, ob[:, :], ALU.add)
                P.dma("pool", "stxm", out_d.ap()[tsl, :], xt[:, :], r=[xt], w=[dx(tt)])
        P.reset("abf", mb)
        P.reset("af", mf)

    for l in range(L):
        if "mix" in phases:
            mix_layer(l)
        if "ffn" in phases:
            ffn_layer(l)

    P.finalize()
    return nc, es, P


def prep_weights(inp, L, ffn=True):
    f = lambda a: np.ascontiguousarray(np.asarray(a, dtype=np.float32))
    w = {}
    w["cst"] = make_consts()
    w["gvec"] = f(np.stack([inp["norm1_g"][:L], inp["norm2_g"][:L], inp["out_g"][:L]], axis=1))
    w["svec"] = f(np.concatenate([inp["sb_qn_g"][:L], inp["sb_kn_g"][:L], inp["sg_vn_g"][:L], inp["ml_i_b"][:L],
                                  inp["ml_f_b"][:L]], axis=1))
    cw = np.asarray(inp["ml_conv_w"][:L]).reshape(L, 4, 6, 128).transpose(0, 3, 2, 1)
    cb = np.asarray(inp["ml_conv_b"][:L]).reshape(L, 6, 128).transpose(0, 2, 1)[..., None]
    w["convw"] = f(np.concatenate([cw, cb], axis=3).reshape(L, 128, 30))
    w["sgw"] = f(np.asarray(inp["sg_w"][:L]).transpose(0, 3, 1, 2).reshape(L, 128, 512))
    w["sgb"] = f(np.asarray(inp["sg_b"][:L]).transpose(0, 2, 1))
    win = np.asarray(inp["w_in"][:L])
    cols_a = np.concatenate([np.arange(0, 1152), np.arange(1920, 2688), np.arange(2700, 3212), np.arange(2688, 2700)])
    cols_b = np.arange(1152, 1920)

    def kmaj(m):
        n = m.shape[2]
        return f(m.reshape(L, 8, 128, n).transpose(0, 2, 1, 3).reshape(L, 128, 8 * n))
    w["wina"] = kmaj(win[:, :, cols_a])
    w["winb"] = kmaj(win[:, :, cols_b])
    w["wout"] = kmaj(np.asarray(inp["w_out"][:L]))
    w["wq"] = kmaj(np.asarray(inp["peer_wq"][:L]))
    w["pkeys"] = f(np.asarray(inp["peer_keys"][:L]).reshape(L, 16, 128, 128).transpose(0, 3, 1, 2).reshape(L, 128, 2048))
    if not ffn:
        return w
    u = np.asarray(inp["peer_u"][:L]).reshape(L, 128, 128, 8, 128)
    w["ut"] = f(u.transpose(0, 2, 4, 3, 1).reshape(L, 128, 128, 1024))
    v = np.asarray(inp["peer_v"][:L]).reshape(L, 128, 128, 1024)
    w["vt"] = f(v.transpose(0, 2, 1, 3))
    return w


_CACHE = {}


def run(inputs, S, L, phases=("mix", "ffn"), ncores=NCORES):
    x = np.asarray(inputs["x"], dtype=np.float32)
    key = (S, L, tuple(phases))
    if key not in _CACHE:
        _CACHE[key] = build(S, L, phases)
    nc, es, P = _CACHE[key]
    w = prep_weights(inputs, L, "ffn" in phases)
    in_maps = []
    for c in range(ncores):
        m = dict(w)
        m["x"] = np.ascontiguousarray(x[c, :S])
        in_maps.append(m)
    res = run_bass_kernel_spmd(nc, in_maps, core_ids=list(range(ncores)))
    return np.stack([np.asarray(r["out"]) for r in res.results], axis=0)


def kernel(**inputs):
    return run(inputs, 4096, 4).astype(np.float32)
```

```python
from contextlib import ExitStack
import numpy as np
import ml_dtypes
import concourse.bass as bass
import concourse.mybir as mybir
from concourse.ap import AP
from concourse.bass_utils import run_bass_kernel_spmd

F32 = mybir.dt.float32
BF16 = mybir.dt.bfloat16
U32 = mybir.dt.uint32
ALU = mybir.AluOpType
AF = mybir.ActivationFunctionType
AX = mybir.AxisListType

D = 1024
NCORES = 8
EPS = 1e-6
NEG = -1.0e30
COLS_A = 2444
OQ, OK_, OV, OMV, OMO, OSG, OMI, OMF = 0, 384, 768, 1152, 1536, 1920, 2432, 2438


class Tile:
    __slots__ = ("ap", "keys")

    def __init__(self, ap, keys):
        self.ap = ap
        self.keys = tuple(keys)

    def __getitem__(self, k):
        return self.ap[k]


class Prog:
    ENGS = ("pe", "act", "dve", "pool", "sp")
    PAGE = 512

    def __init__(self, nc, es):
        self.nc = nc
        self.es = es
        self.ops = []
        self.nkey = 0
        self.arenas = {}
        self.dma_keys = {}

    def key(self):
        self.nkey += 1
        return ("k", self.nkey)

    def static(self, name, shape, dtype):
        t = self.es.enter_context(self.nc.sbuf_tensor("sb_" + name, list(shape), dtype))
        return Tile(t[:], [self.key()])

    def psum(self, name, shape, dtype):
        t = self.es.enter_context(self.nc.psum_tensor(name, list(shape), dtype))
        return Tile(t[:], [self.key()])

    def arena(self, name, nelem, dtype):
        t = self.es.enter_context(self.nc.sbuf_tensor(name, [128, nelem], dtype))
        self.arenas[name] = dict(t=t, n=nelem, off=0)

    def mark(self, name):
        return self.arenas["abf"]["off"]

    def reset(self, name, m):
        self.arenas["abf"]["off"] = m

    def alloc(self, name, shape):
        f32 = (name == "af")
        name = "abf"
        a = self.arenas[name]
        n = int(np.prod(shape))
        nb = 2 * n if f32 else n
        off = a["off"]
        npg = (nb + self.PAGE - 1) // self.PAGE
        assert off + npg * self.PAGE <= a["n"], f"arena {name} overflow need {off + npg * self.PAGE} have {a['n']}"
        a["off"] = off + npg * self.PAGE
        ap = a["t"][:, off:off + nb]
        if f32:
            ap = ap.bitcast(F32)
        if len(shape) == 2:
            ap = ap.rearrange("p (a b) -> p a b", a=shape[0])
        elif len(shape) == 3:
            ap = ap.rearrange("p (a b c) -> p a b c", a=shape[0], b=shape[1])
        keys = [(name, off // self.PAGE + i) for i in range(npg)]
        return Tile(ap, keys)

    def dkey(self, *k):
        return Tile(None, [("d",) + tuple(k)])

    def add(self, eng, fn, r=(), w=(), dma=None):
        import os
        if len(self.ops) >= int(os.environ.get("OPLIMIT", "100000000")):
            return
        rk = []
        for t in r:
            rk.extend(t.keys)
        wk = []
        for t in w:
            wk.extend(t.keys)
        self.ops.append([eng, fn, rk, wk, dma])

    def pos(self, name):
        import os
        if os.environ.get("SHOWPOS"):
            print("POS", name, len(self.ops), flush=True)

    def pe(self, fn, r=(), w=()):
        self.add("pe", fn, r, w)

    def act(self, fn, r=(), w=()):
        self.add("act", fn, r, w)

    def dve(self, fn, r=(), w=()):
        self.add("dve", fn, r, w)

    def pool(self, fn, r=(), w=()):
        self.add("pool", fn, r, w)

    def dma(self, q, key, out, in_, r=(), w=()):
        self.add(q, lambda e: e.dma_start(out=out, in_=in_), r, w, dma=key)

    def finalize(self):
        nc = self.nc
        ops = self.ops
        n = len(ops)
        lastw = {}
        readers = {}
        deps_all = [None] * n
        for k, (eng, fn, rk, wk, dma) in enumerate(ops):
            deps = set()
            for b in rk:
                d = lastw.get(b)
                if d is not None:
                    deps.add(d)
            for b in wk:
                d = lastw.get(b)
                if d is not None:
                    deps.add(d)
                rd = readers.get(b)
                if rd:
                    deps.update(rd)
            deps.discard(k)
            deps_all[k] = deps
            for b in rk:
                readers.setdefault(b, []).append(k)
            for b in wk:
                lastw[b] = k
                readers[b] = []
        waited = {e: {} for e in self.ENGS}
        need = [None] * n
        signal = [False] * n
        for k in range(n):
            eng = ops[k][0]
            wl = waited[eng]
            nd = {}
            for d in deps_all[k]:
                deng, _, _, _, ddma = ops[d]
                if ddma is not None:
                    pk = ("dma", ddma)
                    if wl.get(pk, -1) >= d:
                        continue
                    nd[pk] = max(nd.get(pk, -1), d)
                else:
                    if deng == "pe" and eng == "pe":
                        continue
                    pk = ("eng", deng)
                    if wl.get(pk, -1) >= d:
                        continue
                    nd[pk] = max(nd.get(pk, -1), d)
            for pk, d in nd.items():
                wl[pk] = max(wl.get(pk, -1), d)
                if pk[0] == "eng":
                    signal[d] = True
            need[k] = nd
        sem_eng = {e: self.es.enter_context(nc.semaphore("s_" + e)) for e in self.ENGS}
        cnt = {e: 0 for e in self.ENGS}
        val = [0] * n
        dma_cnt = {}
        dma_idx = {}
        for k in range(n):
            eng, _, _, _, dma = ops[k]
            if dma is not None:
                dma_cnt[dma] = dma_cnt.get(dma, 0) + 1
                dma_idx.setdefault(dma, []).append(k)
                val[k] = 16 * dma_cnt[dma]
            elif signal[k]:
                cnt[eng] += 1
                val[k] = cnt[eng]
        sem_dma = {dk: self.es.enter_context(nc.semaphore("d_%d" % i)) for i, dk in enumerate(dma_cnt)}
        import bisect
        per = {e: [] for e in self.ENGS}
        for k in range(n):
            per[ops[k][0]].append(k)
        final_waits = [(sem_dma[dk], 16 * c) for dk, c in dma_cnt.items()]
        self.stats = {e: len(per[e]) for e in self.ENGS}
        self.stats["signals"] = sum(signal)

        def emit(e, name):
            for k in per[name]:
                _, fn, _, _, dma = ops[k]
                for pk, d in need[k].items():
                    if pk[0] == "eng":
                        e.wait_ge(sem_eng[pk[1]], val[d])
                    else:
                        lst = dma_idx[pk[1]]
                        c = bisect.bisect_left(lst, k)
                        e.wait_ge(sem_dma[pk[1]], 16 * c)
                ins = fn(e)
                if dma is not None:
                    ins.then_inc(sem_dma[dma], 16)
                elif signal[k]:
                    ins.then_inc(sem_eng[name], 1)
            if name == "pool":
                for s, v in final_waits:
                    e.wait_ge(s, v)
                for en in self.ENGS:
                    if en != "pool" and cnt[en] > 0:
                        e.wait_ge(sem_eng[en], cnt[en])

        block = self.es.enter_context(nc.Block())

        @block.sync
        def _(e):
            emit(e, "sp")

        @block.tensor
        def _(e):
            emit(e, "pe")

        @block.scalar
        def _(e):
            emit(e, "act")

        @block.vector
        def _(e):
            emit(e, "dve")

        @block.gpsimd
        def _(e):
            emit(e, "pool")


def bcast_rows(dram_ap_1d_tensor, offset, n):
    return AP(dram_ap_1d_tensor, offset, [[0, 128], [1, n]])


def MM(P, ot, oap, lt, lap, rt, rap, start=True, stop=True):
    P.pe(lambda e: e.matmul(oap, lhsT=lap, rhs=rap, start=start, stop=stop), r=[lt, rt], w=[ot])


def TR(P, ot, oap, it, iap, idt, idap):
    P.pe(lambda e: e.transpose(oap, iap, idap), r=[it, idt], w=[ot])


def ACTV(P, ot, oap, it, iap, func, bias=0.0, scale=1.0, r=(), accum=None, w=()):
    if accum is None:
        P.act(lambda e: e.activation(out=oap, in_=iap, func=func, bias=bias, scale=scale), r=[it] + list(r), w=[ot] + list(w))
    else:
        P.act(lambda e: e.activation(out=oap, in_=iap, func=func, bias=bias, scale=scale, accum_out=accum),
              r=[it] + list(r), w=[ot] + list(w))


def TT(P, eng, ot, oap, at, aap, bt, bap, op):
    P.add(eng, lambda e: e.tensor_tensor(out=oap, in0=aap, in1=bap, op=op), r=[at, bt], w=[ot])


def TS(P, eng, ot, oap, at, aap, s1, s2, op0, op1=None, r=()):
    if op1 is None:
        P.add(eng, lambda e: e.tensor_scalar(out=oap, in0=aap, scalar1=s1, scalar2=None, op0=op0), r=[at] + list(r), w=[ot])
    else:
        P.add(eng, lambda e: e.tensor_scalar(out=oap, in0=aap, scalar1=s1, scalar2=s2, op0=op0, op1=op1),
              r=[at] + list(r), w=[ot])


def STT(P, ot, oap, at, aap, sc, bt, bap, op0, op1, r=()):
    P.dve(lambda e: e.scalar_tensor_tensor(out=oap, in0=aap, scalar=sc, in1=bap, op0=op0, op1=op1),
          r=[at, bt] + list(r), w=[ot])


def CP(P, eng, ot, oap, it, iap):
    if eng == "act":
        P.act(lambda e: e.copy(out=oap, in_=iap), r=[it], w=[ot])
    else:
        P.add(eng, lambda e: e.tensor_copy(out=oap, in_=iap), r=[it], w=[ot])


def RED(P, ot, oap, it, iap, op, axis=None):
    axis = AX.X if axis is None else axis
    P.dve(lambda e: e.tensor_reduce(out=oap, in_=iap, axis=axis, op=op), r=[it], w=[ot])


def bc(ap, axis, n):
    a = ap.unsqueeze(axis)
    shp = list(a.shape)
    shp[axis] = n
    return a.broadcast_to(shp)


C_IDF, C_IOTA, C_IOTA16, C_THR, C_TRI, C_NTRI, C_NONE, C_MASK4 = 0, 128, 256, 272, 288, 416, 544, 672
NCST = 672 + 2048


def make_consts():
    c = np.zeros((128, NCST), np.float32)
    c[:, C_IDF:C_IDF + 128] = np.eye(128, dtype=np.float32)
    c[:, C_IOTA:C_IOTA + 128] = np.arange(128, dtype=np.float32)[None, :]
    c[:, C_IOTA16:C_IOTA16 + 16] = np.arange(16, dtype=np.float32)[None, :]
    c[:, C_THR:C_THR + 16] = (16.0 * np.arange(1, 17, dtype=np.float32))[None, :]
    j = np.arange(128)[:, None]
    t = np.arange(128)[None, :]
    c[:, C_TRI:C_TRI + 128] = (j <= t).astype(np.float32)
    c[:, C_NTRI:C_NTRI + 128] = -(j >= t).astype(np.float32)
    c[:, C_NONE:C_NONE + 128] = -1.0
    q = np.arange(512)[None, :]
    for r in range(4):
        c[:, C_MASK4 + r * 512:C_MASK4 + (r + 1) * 512] = ((128 * r + j) < q).astype(np.float32)
    return c


def build(S, L, phases=("mix", "ffn"), dbg=False):
    NT = S // 128
    nc = bass.Bass("TRN2", target_bir_lowering=False)
    es = ExitStack()
    P = Prog(nc, es)

    def din(name, shape):
        return nc.dram_tensor(name, list(shape), F32, kind="ExternalInput")

    x_d = din("x", [S, D])
    cst_d = din("cst", [128, NCST])
    gvec_d = din("gvec", [L, 3, D])
    svec_d = din("svec", [L, 396])
    convw_d = din("convw", [L, 128, 30])
    sgw_d = din("sgw", [L, 128, 512])
    sgb_d = din("sgb", [L, 128, 4])
    wina_d = din("wina", [L, 128, 8 * COLS_A])
    winb_d = din("winb", [L, 128, 8 * 768])
    wout_d = din("wout", [L, 128, 8 * 1024])
    wq_d = din("wq", [L, 128, 8 * 2048])
    keys_d = din("pkeys", [L, 128, 16 * 128])
    if "ffn" in phases:
        ut_d = din("ut", [L, 128, 128, 1024])
        vt_d = din("vt", [L, 128, 128, 1024])
    out_d = nc.dram_tensor("out", [S, D], F32, kind="ExternalOutput")
    ubf_d = nc.dram_tensor("ubf", [L, 128, 128, 1024], BF16, kind="Internal")
    vbf_d = nc.dram_tensor("vbf", [L, 128, 128, 1024], BF16, kind="Internal")
    yms_d = nc.dram_tensor("yms", [S, 640], BF16, kind="Internal")
    dbg_d = nc.dram_tensor("dbg", [S, D], F32, kind="ExternalOutput") if dbg else None

    P.arena("abf", 168 * 512, BF16)

    ps = [P.psum("ps%d" % i, [128, 512], F32) for i in range(7)]
    psT = P.psum("psT", [128, 1024], BF16)
    psTf = Tile(psT.ap.bitcast(F32), psT.keys)

    cstf = P.static("cstf", [128, C_MASK4], F32)
    P.dma("sp", "cst", cstf[:, :], cst_d.ap()[:, 0:C_MASK4], w=[cstf])
    identb = P.static("identb", [128, 128], BF16)
    iotab = P.static("iotab", [128, 128], BF16)
    CP(P, "dve", identb, identb[:, :], cstf, cstf[:, C_IDF:C_IDF + 128])
    CP(P, "dve", iotab, iotab[:, :], cstf, cstf[:, C_IOTA:C_IOTA + 128])
    identf = Tile(cstf[:, C_IDF:C_IDF + 128], cstf.keys)

    svt = P.static("svt", [128, 396], F32)
    stg = [P.static("stg%d" % i, [128, 1024], F32) for i in range(2)]
    cbf = [P.static("cbf%d" % i, [128, 1024], BF16) for i in range(2)]
    cnt = {"stg": 0, "cast": 0}

    def dx(tt):
        return P.dkey("x", tt)

    for tt in range(NT):
        P.dma("sp", "xcp", out_d.ap()[tt * 128:(tt + 1) * 128, :], x_d.ap()[tt * 128:(tt + 1) * 128, :], w=[dx(tt)])

    def load_cast(dst_t, dst_ap2, src_ap2, n):
        for c0 in range(0, n, 1024):
            w_ = min(1024, n - c0)
            i = cnt["stg"] % 2
            cnt["stg"] += 1
            P.dma("sp", "stg%d" % i, stg[i][:, 0:w_], src_ap2[:, c0:c0 + w_], w=[stg[i]])
            eng = ("dve", "act")[cnt["cast"] % 2]
            cnt["cast"] += 1
            CP(P, eng, dst_t, dst_ap2[:, c0:c0 + w_], stg[i], stg[i][:, 0:w_])

    def rstd_of(ss_t, ss_ap, n_inv, out_t, out_ap, tmp_t, tmp_ap):
        TS(P, "dve", tmp_t, tmp_ap, ss_t, ss_ap, n_inv, EPS, ALU.mult, ALU.add)
        ACTV(P, tmp_t, tmp_ap, tmp_t, tmp_ap, AF.Ln)
        ACTV(P, out_t, out_ap, tmp_t, tmp_ap, AF.Exp, scale=-0.5)

    def norm_transpose(xt, gt, xn, xnT_t, xnT_ap3, ss, tmp):
        ACTV(P, xn, xn[:, :], xt, xt[:, :], AF.Square, accum=ss[:, 0:1], w=[ss])
        rstd_of(ss, ss[:, 0:1], 1.0 / D, ss, ss[:, 1:2], tmp, tmp[:, 0:1])
        STT(P, xn, xn[:, :], xt, xt[:, :], ss[:, 1:2], gt, gt[:, :], ALU.mult, ALU.mult, r=[ss])
        for kc in range(8):
            TR(P, psT, psT[:, kc * 128:(kc + 1) * 128], xn, xn[:, kc * 128:(kc + 1) * 128], identb, identb[:, :])
        CP(P, "act", xnT_t, xnT_ap3, psT, psT[:, :].rearrange("p (a b) -> p a b", a=8))


    ffn_st = (P.static("v16", [128, 16, 16], F32), P.static("i16u", [128, 16, 16], U32), P.static("i16f", [128, 16, 16], F32),
              P.static("c16", [128, 8, 16], F32), P.static("p16u", [128, 8, 16], U32), P.static("p16f", [128, 8, 16], F32),
              P.static("kq", [128, 8, 16], F32), P.static("kr", [128, 8, 16], F32), P.static("isel", [128, 8, 16], F32),
              P.static("jsel", [128, 8, 16], F32), P.static("gsel", [128, 8, 16], F32), P.static("smf", [128, 32], F32),
              P.static("fss", [128, 4], F32))

    wqbf_d = nc.dram_tensor("wqbf", [L, 128, 8 * 2048], BF16, kind="Internal") if "ffn" in phases else None

    def ffn_cast_gen(l):
        def one(src_ap, dst_ap, n, dk):
            i = cnt["stg"] % 2
            cnt["stg"] += 1
            P.dma("sp", "stg%d" % i, stg[i][:, 0:n], src_ap, w=[stg[i]])
            eng = ("dve", "act")[cnt["cast"] % 2]
            cnt["cast"] += 1
            CP(P, eng, cbf[i], cbf[i][:, 0:n], stg[i], stg[i][:, 0:n])
            P.dma("pool", "cbf%d" % i, dst_ap, cbf[i][:, 0:n], r=[cbf[i]], w=[dk])
        for c in range(16):
            one(wq_d.ap()[l][:, c * 1024:(c + 1) * 1024], wqbf_d.ap()[l][:, c * 1024:(c + 1) * 1024], 1024, P.dkey("wqbf", l))
            yield
        for j in range(128):
            one(ut_d.ap()[l, j], ubf_d.ap()[l, j], 1024, P.dkey("ubf", l, j))
            yield
            one(vt_d.ap()[l, j], vbf_d.ap()[l, j], 1024, P.dkey("vbf", l, j))
            yield

    def ffn_layer(l):
        mb, mf = P.mark("abf"), P.mark("af")
        TB = 256 if S >= 256 else 128
        NTB = TB // 128
        NBLK = S // TB
        (v16, i16u, i16f, c16, p16u, p16f, kq, kr, isel, jsel, gsel, sm, ss) = ffn_st
        keys_sb = P.alloc("abf", (16, 128))
        xn = P.alloc("abf", (D,))
        xnT2 = [P.alloc("abf", (8, TB)) for _ in range(2)]
        qT = P.alloc("abf", (16, TB))
        selT2 = [P.alloc("abf", (3, TB)) for _ in range(2)]
        OIg = P.alloc("abf", (16, 128))
        OJ = P.alloc("abf", (16, 128))
        W_sb = P.alloc("abf", (TB, 128))
        NR = 4
        u_r = [P.alloc("abf", (8, 128)) for _ in range(NR)]
        v_r = [P.alloc("abf", (D,)) for _ in range(NR)]
        NH = 3
        Hh = [P.alloc("abf", (TB,)) for _ in range(NH)]
        npb = 512 // TB
        wqs = [P.alloc("abf", (8, npb * 128)) for _ in range(2)]
        xres2 = [[P.alloc("af", (D,)) for _ in range(NTB)] for _ in range(2)]
        sc = P.alloc("af", (16, 128))
        sc2 = P.alloc("af", (16, 128))
        Gs = [P.alloc("af", (TB,)) for _ in range(NH)]
        g2t = P.alloc("af", (D,))
        cand = Tile(sc.ap.rearrange("p a b -> p (a b)").rearrange("p (h x) -> p h x", h=8), sc.keys)
        cand2 = Tile(sc2.ap.rearrange("p a b -> p (a b)").rearrange("p (h x) -> p h x", h=8), sc2.keys)
        oh = Tile(sc2.ap.rearrange("p a b -> p (a b)").rearrange("p (h r k) -> p h r k", h=8, r=16), sc2.keys)

        P.dma("sp", "g2t", g2t[:, :], bcast_rows(gvec_d, (l * 3 + 1) * D, D), w=[g2t])
        load_cast(keys_sb, keys_sb.ap.rearrange("p a b -> p (a b)"), keys_d.ap()[l], 16 * 128)

        iota3 = AP(iotab.ap.tensor, iotab.ap.offset, [list(iotab.ap.ap[0]), [0, 16], [1, 128]])
        iota16_4 = AP(cstf.ap.tensor, cstf.ap.offset + C_IOTA16, [list(cstf.ap.ap[0]), [0, 8], [0, 16], [1, 16]])
        thr_4 = AP(cstf.ap.tensor, cstf.ap.offset + C_THR, [list(cstf.ap.ap[0]), [0, 8], [0, 16], [1, 16]])
        misc = [ps[6], psTf]
        mcnt = [0]
        wcnt = [0]
        wq3 = wqbf_d.ap()[l].rearrange("p (k c) -> p k c", k=8)

        def mbank():
            b = misc[mcnt[0] % 2]
            mcnt[0] += 1
            return b

        def f_sel(blk):
            par = blk % 2
            xres, xnT, selT = xres2[par], xnT2[par], selT2[par]
            for ti in range(NTB):
                tt = blk * NTB + ti
                P.dma("sp", "xres%d_%d" % (par, ti), xres[ti][:, :], out_d.ap()[tt * 128:(tt + 1) * 128, :], r=[dx(tt)], w=[xres[ti]])
                norm_transpose(xres[ti], g2t, xn, xnT, xnT[:, :, ti * 128:(ti + 1) * 128], ss, sm)
                yield
            for h0 in range(0, 16, npb):
                ws = wqs[wcnt[0] % 2]
                P.dma("sp", "wqs%d" % (wcnt[0] % 2), ws[:, :, :], wq3[:, :, h0 * 128:(h0 + npb) * 128], r=[P.dkey("wqbf", l)], w=[ws])
                wcnt[0] += 1
                b = mbank()
                for s_ in range(npb):
                    for kc in range(8):
                        MM(P, b, b[:, s_ * TB:(s_ + 1) * TB], ws, ws[:, kc, s_ * 128:(s_ + 1) * 128],
                           xnT, xnT[:, kc, :], start=(kc == 0), stop=(kc == 7))
                CP(P, "act", qT, qT[:, h0:h0 + npb, :], b, b[:, :].rearrange("p (a b) -> p a b", a=npb))
                yield
            for ti in range(NTB):
                tsl = slice(ti * 128, (ti + 1) * 128)
                for q4 in range(4):
                    b = mbank()
                    for s4 in range(4):
                        hc = 4 * q4 + s4
                        MM(P, b, b[:, s4 * 128:(s4 + 1) * 128], qT, qT[:, hc, tsl], keys_sb, keys_sb[:, hc, :])
                    CP(P, "act", sc, sc[:, 4 * q4:4 * q4 + 4, :], b, b[:, :].rearrange("p (a b) -> p a b", a=4))
                    yield
                for hc in range(16):
                    P.dve(lambda e, hc=hc: e.max(out=v16[:, hc, 0:8], in_=sc[:, hc, :]), r=[sc], w=[v16])
                    P.dve(lambda e, hc=hc: e.max_index(out=i16u[:, hc, 0:8], in_max=v16[:, hc, 0:8], in_values=sc[:, hc, :]),
                          r=[sc, v16], w=[i16u])
                    P.dve(lambda e, hc=hc: e.match_replace(out=sc2[:, hc, :], in_to_replace=v16[:, hc, 0:8],
                                                           in_values=sc[:, hc, :], imm_value=NEG), r=[sc, v16], w=[sc2])
                    P.dve(lambda e, hc=hc: e.max(out=v16[:, hc, 8:16], in_=sc2[:, hc, :]), r=[sc2], w=[v16])
                    P.dve(lambda e, hc=hc: e.max_index(out=i16u[:, hc, 8:16], in_max=v16[:, hc, 8:16], in_values=sc2[:, hc, :]),
                          r=[sc2, v16], w=[i16u])
                    yield
                CP(P, "dve", i16f, i16f[:, :, :], i16u, i16u[:, :, :])
                v4 = v16.ap.rearrange("p (h c) k -> p h c k", c=2)
                i4 = i16f.ap.rearrange("p (h c) k -> p h c k", c=2)
                TT(P, "dve", cand, cand.ap.rearrange("p h (a b) -> p h a b", a=16), v16, bc(v4[:, :, 0, :], 3, 16),
                   v16, bc(v4[:, :, 1, :], 2, 16), ALU.add)
                for h in range(8):
                    P.dve(lambda e, h=h: e.max(out=c16[:, h, 0:8], in_=cand[:, h, :]), r=[cand], w=[c16])
                    P.dve(lambda e, h=h: e.max_index(out=p16u[:, h, 0:8], in_max=c16[:, h, 0:8], in_values=cand[:, h, :]),
                          r=[cand, c16], w=[p16u])
                    P.dve(lambda e, h=h: e.match_replace(out=cand2[:, h, :], in_to_replace=c16[:, h, 0:8],
                                                         in_values=cand[:, h, :], imm_value=NEG), r=[cand, c16], w=[cand2])
                    P.dve(lambda e, h=h: e.max(out=c16[:, h, 8:16], in_=cand2[:, h, :]), r=[cand2], w=[c16])
                    P.dve(lambda e, h=h: e.max_index(out=p16u[:, h, 8:16], in_max=c16[:, h, 8:16], in_values=cand2[:, h, :]),
                          r=[cand2, c16], w=[p16u])
                    yield
                CP(P, "dve", p16f, p16f[:, :, :], p16u, p16u[:, :, :])
                TT(P, "dve", oh, oh[:, :, :, :], p16f, bc(p16f[:, :, :], 3, 16), cstf, thr_4, ALU.is_ge)
                RED(P, kq, kq[:, :, :], oh, oh[:, :, :, :], ALU.add)
                STT(P, kr, kr[:, :, :], kq, kq[:, :, :], -16.0, p16f, p16f[:, :, :], ALU.mult, ALU.add)
                yield
                for (kk, src, dst) in ((kq, i4[:, :, 0, :], isel), (kr, i4[:, :, 1, :], jsel)):
                    TT(P, "dve", oh, oh[:, :, :, :], kk, bc(kk[:, :, :], 3, 16), cstf, iota16_4, ALU.is_equal)
                    TT(P, "dve", oh, oh[:, :, :, :], oh, oh[:, :, :, :], i16f, bc(src, 2, 16), ALU.mult)
                    RED(P, dst, dst[:, :, :], oh, oh[:, :, :, :], ALU.add)
                    yield
                RED(P, sm, sm[:, 0:8], c16, c16[:, :, :], ALU.max)
                TT(P, "dve", gsel, gsel[:, :, :], c16, c16[:, :, :], sm, bc(sm[:, 0:8], 2, 16), ALU.subtract)
                ACTV(P, gsel, gsel[:, :, :], gsel, gsel[:, :, :], AF.Exp)
                RED(P, sm, sm[:, 8:16], gsel, gsel[:, :, :], ALU.add)
                P.dve(lambda e: e.reciprocal(out=sm[:, 16:24], in_=sm[:, 8:16]), r=[sm], w=[sm])
                TT(P, "dve", gsel, gsel[:, :, :], gsel, gsel[:, :, :], sm, bc(sm[:, 16:24], 2, 16), ALU.mult)
                yield
                b = mbank()
                for q_, src in enumerate((isel, jsel, gsel)):
                    TR(P, b, b[:, q_ * 128:(q_ + 1) * 128], src, src.ap.rearrange("p h r -> p (h r)"), identf, identf[:, :])
                CP(P, "act", selT, selT[:, :, tsl], b, b[:, 0:384].rearrange("p (a b) -> p a b", a=3))
                yield

        def f_w(blk):
            selT = selT2[blk % 2]
            for tb in range(TB // 16):
                t0 = tb * 16
                TT(P, "dve", OIg, OIg[:, :, :], iotab, iota3, selT, bc(selT[:, 0, t0:t0 + 16], 2, 128), ALU.is_equal)
                TT(P, "pool", OIg, OIg[:, :, :], OIg, OIg[:, :, :], selT, bc(selT[:, 2, t0:t0 + 16], 2, 128), ALU.mult)
                TT(P, "dve", OJ, OJ[:, :, :], iotab, iota3, selT, bc(selT[:, 1, t0:t0 + 16], 2, 128), ALU.is_equal)
                for q_ in range(4):
                    b = mbank()
                    for u_ in range(4):
                        t = q_ * 4 + u_
                        MM(P, b, b[:, u_ * 128:(u_ + 1) * 128], OIg, OIg[:, t, :], OJ, OJ[:, t, :])
                    CP(P, ("act", "dve")[q_ % 2], W_sb, W_sb[:, t0 + q_ * 4:t0 + q_ * 4 + 4, :], b,
                       b[:, :].rearrange("p (a b) -> p a b", a=4))

        def f_main(blk, gen):
            xnT = xnT2[blk % 2]
            for j in range(128):
                sl = j % NR
                P.dma("sp", "u%d" % sl, u_r[sl].ap.rearrange("p a b -> p (a b)"), ubf_d.ap()[l, j],
                      r=[P.dkey("ubf", l, j)], w=[u_r[sl]])
                P.dma("sp", "v%d" % sl, v_r[sl][:, :], vbf_d.ap()[l, j], r=[P.dkey("vbf", l, j)], w=[v_r[sl]])
                ab = ps[4 + (j % 2)]
                for kc in range(8):
                    MM(P, ab, ab[:, 0:TB], u_r[sl], u_r[sl][:, kc, :], xnT, xnT[:, kc, :], start=(kc == 0), stop=(kc == 7))
                g_ = Gs[j % NH]
                h_ = Hh[j % NH]
                ACTV(P, g_, g_[:, :], ab, ab[:, 0:TB], AF.Gelu_apprx_tanh)
                TT(P, "dve", h_, h_[:, :], g_, g_[:, :], W_sb, W_sb[:, :, j], ALU.mult)
                for ti in range(NTB):
                    for hf in range(2):
                        yb = ps[ti * 2 + hf]
                        MM(P, yb, yb[:, :], h_, h_[:, ti * 128:(ti + 1) * 128], v_r[sl], v_r[sl][:, hf * 512:(hf + 1) * 512],
                           start=(j == 0), stop=(j == 127))
                if gen is not None and j >= 4:
                    next(gen, None)
            if gen is not None:
                for _ in gen:
                    pass

        def f_out(blk):
            xres = xres2[blk % 2]
            for ti in range(NTB):
                tt = blk * NTB + ti
                for hf in range(2):
                    yb = ps[ti * 2 + hf]
                    TT(P, "dve", xres[ti], xres[ti][:, hf * 512:(hf + 1) * 512], xres[ti], xres[ti][:, hf * 512:(hf + 1) * 512],
                       yb, yb[:, :], ALU.add)
                P.dma("pool", "stx%d_%d" % (blk % 2, ti), out_d.ap()[tt * 128:(tt + 1) * 128, :], xres[ti][:, :], r=[xres[ti]], w=[dx(tt)])

        for _ in f_sel(0):
            pass
        for blk in range(NBLK):
            f_w(blk)
            f_main(blk, f_sel(blk + 1) if blk + 1 < NBLK else None)
            f_out(blk)
        P.reset("abf", mb)
        P.reset("af", mf)

    QG = min(512, S)
    TG = QG // 128
    qT_d = nc.dram_tensor("qTd", [NT, 128, 384], BF16, kind="Internal")
    trib = P.static("trib", [128, 128], BF16)
    ntrib = P.static("ntrib", [128, 128], BF16)
    noneb = P.static("noneb", [128, 128], BF16)
    CP(P, "dve", trib, trib[:, :], cstf, cstf[:, C_TRI:C_TRI + 128])
    CP(P, "dve", ntrib, ntrib[:, :], cstf, cstf[:, C_NTRI:C_NTRI + 128])
    CP(P, "dve", noneb, noneb[:, :], cstf, cstf[:, C_NONE:C_NONE + 128])
    trif = Tile(cstf[:, C_TRI:C_TRI + 128], cstf.keys)
    cw = P.static("cw", [128, 6, 5], F32)
    sgb = P.static("sgb", [128, 4], F32)
    Cn = P.static("Cn", [128, 3, 65], F32)
    Cb = P.static("Cb", [128, 3, 65], BF16)
    cvin = P.static("cvin", [128, 6, 131], F32)
    v1 = P.static("v1", [128, 6, 65], BF16)
    sm2 = P.static("sm2", [128, 64], F32)
    ss2 = P.static("ss2", [128, 4], F32)
    gif = P.static("gif", [128, 12], F32)
    nlf = P.static("nlf", [128, 6], F32)
    nb = P.static("nb", [128, 6], F32)
    nhl = P.static("nhl", [128, 2, 6], BF16)
    nh32 = P.static("nh32", [128, 6], F32)
    nbh = P.static("nbh", [128, 2, 6, 128], BF16)
    nbp = P.static("nbp", [128, 2, 3, 128], BF16)
    bD = P.static("bD", [128, 6], F32)
    wsv = P.static("wsv", [128, 6], F32)
    dec = P.static("dec", [128, 3], F32)
    one_t = P.static("one_t", [128, 1], F32)
    P.dve(lambda e: e.memset(one_t[:, :], 1.0), w=[one_t])

    def mix_layer(l, cg=None):
        def pump(n):
            if cg is not None:
                for _ in range(n):
                    next(cg, None)

        mb, mf = P.mark("abf"), P.mark("af")
        wout = P.alloc("abf", (8, 1024))
        kTa = P.alloc("abf", (3, S))
        va = P.alloc("abf", (NT, 384))
        g1t_ = P.alloc("af", (D,))
        ogt_ = P.alloc("af", (D,))
        xt = P.alloc("af", (D,))
        mb1, mf1 = P.mark("abf"), P.mark("af")
        winA = P.alloc("abf", (8, COLS_A))
        winB = P.alloc("abf", (8, 768))
        sgw = P.alloc("abf", (4, 128))
        xn = P.alloc("abf", (D,))
        xnT = P.alloc("abf", (8, 128))
        qkb = P.alloc("abf", (768,))
        mqT = P.alloc("abf", (3, 128))
        mkT = P.alloc("abf", (3, 128))
        qsTz = P.alloc("abf", (6, 128))
        ebp = P.alloc("abf", (3, 128))
        mktok = P.alloc("abf", (384,))
        kw = P.alloc("abf", (384,))
        PT = P.alloc("abf", (6, 128))
        mqTz = Tile(PT.ap, PT.keys)
        og = P.alloc("abf", (384,))
        ymsb = P.alloc("abf", (640,))
        sgvb = P.alloc("abf", (256,))
        qst = P.alloc("abf", (384,))
        zq = P.alloc("af", (768,))
        uv = P.alloc("af", (512,))
        cva = P.alloc("af", (6, 128))
        yb = P.alloc("af", (D,))
        DTf = Tile(cva.ap, cva.keys)

        P.dma("sp", "g1t", g1t_[:, :], bcast_rows(gvec_d, (l * 3 + 0) * D, D), w=[g1t_])
        P.dma("sp", "ogt", ogt_[:, :], bcast_rows(gvec_d, (l * 3 + 2) * D, D), w=[ogt_])
        P.dma("sp", "svt", svt[:, :], bcast_rows(svec_d, l * 396, 396), w=[svt])
        P.dma("sp", "cw", cw.ap.rearrange("p a b -> p (a b)"), convw_d.ap()[l], w=[cw])
        P.dma("sp", "sgb", sgb[:, :], sgb_d.ap()[l], w=[sgb])
        load_cast(winA, winA.ap.rearrange("p a b -> p (a b)"), wina_d.ap()[l], 8 * COLS_A)
        load_cast(winB, winB.ap.rearrange("p a b -> p (a b)"), winb_d.ap()[l], 8 * 768)
        load_cast(wout, wout.ap.rearrange("p a b -> p (a b)"), wout_d.ap()[l], 8 * 1024)
        P.dma("sp", "stg0", stg[0][:, 0:512], sgw_d.ap()[l], w=[stg[0]])
        TT(P, "dve", sgw, sgw[:, :, :], stg[0], stg[0][:, 0:512].rearrange("p (g t) -> p g t", g=4), cstf,
           bc(cstf[:, C_TRI:C_TRI + 128], 1, 4), ALU.mult)
        P.dve(lambda e: e.memset(Cn[:, :, :], 0.0), w=[Cn])
        P.dve(lambda e: e.memset(Cb[:, :, :], 0.0), w=[Cb])
        P.dve(lambda e: e.memset(cvin[:, :, 0:3], 0.0), w=[cvin])
        P.dve(lambda e: e.memset(v1[:, :, 64:65], 1.0), w=[v1])

        chunks = [(0, 512), (512, 512), (1024, 512), (1536, 512), (2048, 396)]
        for tt in range(NT):
            tsl = slice(tt * 128, (tt + 1) * 128)
            P.dma("sp", "xt", xt[:, :], out_d.ap()[tsl, :], r=[dx(tt)], w=[xt])
            norm_transpose(xt, g1t_, xn, xnT, xnT[:, :, :], ss2, sm2)
            pump(max(1, 160 // NT))
            for ci, (c0, cw_) in enumerate(chunks):
                b = ps[ci % 2]
                for kc in range(8):
                    MM(P, b, b[:, 0:cw_], xnT, xnT[:, kc, :], winA, winA[:, kc, c0:c0 + cw_], start=(kc == 0), stop=(kc == 7))
                if ci == 0:
                    CP(P, "act", zq, zq[:, 0:512], b, b[:, 0:512])
                elif ci == 1:
                    CP(P, "act", zq, zq[:, 512:768], b, b[:, 0:256])
                    CP(P, "act", va, va[:, tt, 0:256], b, b[:, 256:512])
                elif ci == 2:
                    CP(P, "act", va, va[:, tt, 256:384], b, b[:, 0:128])
                    CP(P, "act", v1, v1[:, :, 0:64], b, b[:, 128:512].rearrange("p (h d) -> p h d", h=6))
                elif ci == 3:
                    ACTV(P, og, og[:, :], b, b[:, 0:384], AF.Sigmoid)
                    ACTV(P, uv, uv[:, 0:128], b, b[:, 384:512], AF.Gelu_apprx_tanh)
                else:
                    ACTV(P, uv, uv[:, 128:512], b, b[:, 0:384], AF.Gelu_apprx_tanh)
                    TT(P, "dve", gif, gif[:, :], b, b[:, 384:396], svt, svt[:, 384:396], ALU.add)
            P.pos("zT")
            for c in range(6):
                b = ps[2] if c < 4 else ps[3]
                o_ = (c % 4) * 128
                for kc in range(8):
                    MM(P, b, b[:, o_:o_ + 128], winB, winB[:, kc, c * 128:(c + 1) * 128], xnT, xnT[:, kc, :],
                       start=(kc == 0), stop=(kc == 7))
            CP(P, "act", cvin, cvin[:, 0:4, 3:131], ps[2], ps[2][:, :].rearrange("p (a b) -> p a b", a=4))
            CP(P, "act", cvin, cvin[:, 4:6, 3:131], ps[3], ps[3][:, 0:256].rearrange("p (a b) -> p a b", a=2))
            P.pos("sbnorm")
            z12 = zq.ap.rearrange("p (h d) -> p h d", h=12)
            y12 = yb.ap[:, 0:768].rearrange("p (h d) -> p h d", h=12)
            TT(P, "dve", yb, y12, zq, z12, zq, z12, ALU.mult)
            RED(P, sm2, sm2[:, 0:12], yb, y12, ALU.add)
            rstd_of(sm2, sm2[:, 0:12], 1.0 / 64, sm2, sm2[:, 16:28], sm2, sm2[:, 32:44])
            TS(P, "dve", sm2, sm2[:, 16:22], sm2, sm2[:, 16:22], 0.125, None, ALU.mult)
            TT(P, "dve", zq, z12, zq, z12, sm2, bc(sm2[:, 16:28], 2, 64), ALU.mult)
            gqk = AP(svt.ap.tensor, svt.ap.offset, [list(svt.ap.ap[0]), [64, 2], [0, 6], [1, 64]])
            TT(P, "dve", qkb, qkb.ap.rearrange("p (c h d) -> p c h d", c=2, h=6), zq,
               zq.ap.rearrange("p (c h d) -> p c h d", c=2, h=6), svt, gqk, ALU.mult)
            for p_ in range(6):
                TR(P, psT, psT[:, p_ * 128:(p_ + 1) * 128], qkb, qkb[:, p_ * 128:(p_ + 1) * 128], identb, identb[:, :])
            CP(P, "act", qst, qst[:, :], psT, psT[:, 0:384])
            P.dma("pool", "qst", qT_d.ap()[tt], qst[:, :], r=[qst], w=[P.dkey("qT", tt)])
            CP(P, "act", kTa, kTa[:, :, tsl], psT, psT[:, 384:768].rearrange("p (a b) -> p a b", a=3))
            P.pos("conv")
            y6 = yb.ap[:, 0:768].rearrange("p (c t) -> p c t", c=6)
            TT(P, "dve", cva, cva[:, :, :], cvin, cvin[:, :, 0:128], cw, bc(cw[:, :, 0], 2, 128), ALU.mult)
            TT(P, "dve", cva, cva[:, :, :], cva, cva[:, :, :], cw, bc(cw[:, :, 4], 2, 128), ALU.add)
            for j in range(1, 4):
                TT(P, "dve", yb, y6, cvin, cvin[:, :, j:j + 128], cw, bc(cw[:, :, j], 2, 128), ALU.mult)
                TT(P, "dve", cva, cva[:, :, :], cva, cva[:, :, :], yb, y6, ALU.add)
            CP(P, "dve", cvin, cvin[:, :, 0:3], cvin, cvin[:, :, 128:131])
            ACTV(P, yb, y6, cva, cva[:, :, :], AF.Sigmoid)
            TT(P, "dve", mqT, mqT[:, :, :], cva, cva[:, 0:3, :], yb, y6[:, 0:3, :], ALU.mult)
            STT(P, mkT, mkT[:, :, :], cva, cva[:, 3:6, :], 0.125, yb, y6[:, 3:6, :], ALU.mult, ALU.mult)
            for p_ in range(3):
                TR(P, psT, psT[:, p_ * 128:(p_ + 1) * 128], mkT, mkT[:, p_, :], identb, identb[:, :])
            CP(P, "act", mktok, mktok[:, :], psT, psT[:, 0:384])
            P.pos("gates")
            ACTV(P, nlf, nlf[:, :], gif, gif[:, 6:12], AF.Exp, scale=-1.0)
            ACTV(P, nlf, nlf[:, :], nlf, nlf[:, :], AF.Ln, bias=one_t[:, 0:1], r=[one_t])
            b6 = ps[6]
            CP(P, "dve", nhl, nhl[:, 0, :], nlf, nlf[:, :])
            CP(P, "dve", nh32, nh32[:, :], nhl, nhl[:, 0, :])
            TT(P, "dve", nhl, nhl[:, 1, :], nlf, nlf[:, :], nh32, nh32[:, :], ALU.subtract)
            CP(P, "dve", nbh, nbh[:, :, :, :], nhl, bc(nhl[:, :, :], 3, 128))
            CP(P, "dve", nbp, nbp.ap.rearrange("p c a (b d) -> p (c a) b d", b=2), nhl,
               bc(nhl.ap.rearrange("p c (a b) -> p (c a) b", a=3), 3, 64))
            for h in range(6):
                b = ps[4] if h < 4 else ps[5]
                o_ = (h % 4) * 128
                for c_ in range(2):
                    MM(P, b, b[:, o_:o_ + 128], nbh, nbh[:, c_, h, :], trib, trib[:, :], start=(c_ == 0), stop=(c_ == 1))
            for p_ in range(3):
                for c_ in range(2):
                    MM(P, b6, b6[:, p_ * 128:(p_ + 1) * 128], nbp, nbp[:, c_, p_, :], trib, trib[:, :], start=(c_ == 0), stop=(c_ == 1))
            yd = yb.ap[:, 0:768].rearrange("p (h t) -> p h t", h=6)
            TT(P, "dve", yb, yd[:, 0:4, :], ps[4], ps[4][:, :].rearrange("p (a b) -> p a b", a=4), cstf, bc(cstf[:, C_IDF:C_IDF + 128], 1, 4), ALU.mult)
            TT(P, "dve", yb, yd[:, 4:6, :], ps[5], ps[5][:, 0:256].rearrange("p (a b) -> p a b", a=2), cstf, bc(cstf[:, C_IDF:C_IDF + 128], 1, 2), ALU.mult)
            RED(P, nb, nb[:, :], yb, yd, ALU.add)
            TT(P, "dve", bD, bD[:, :], nb, nb[:, :], gif, gif[:, 0:6], ALU.add)
            for h in range(6):
                b = ps[4] if h < 4 else ps[5]
                o_ = (h % 4) * 128
                ACTV(P, DTf, DTf[:, h, :], b, b[:, o_:o_ + 128], AF.Exp, bias=bD[:, h:h + 1], scale=-1.0, r=[bD])
            TT(P, "dve", DTf, DTf[:, :, :], DTf, DTf[:, :, :], cstf, bc(cstf[:, C_TRI:C_TRI + 128], 1, 6), ALU.mult)
            ACTV(P, ebp, ebp[:, :, :], b6, b6[:, 0:384].rearrange("p (a b) -> p a b", a=3), AF.Exp, scale=-1.0)
            ACTV(P, dec, dec[:, :], b6, b6[:, 0:384].rearrange("p (a b) -> p a b", a=3)[:, :, 127], AF.Exp, scale=-1.0)
            TT(P, "dve", wsv, wsv[:, 0:4], bD, bD[:, 0:4], ps[4], ps[4][:, :].rearrange("p (a b) -> p a b", a=4)[:, :, 127],
               ALU.subtract)
            TT(P, "dve", wsv, wsv[:, 4:6], bD, bD[:, 4:6], ps[5], ps[5][:, 0:256].rearrange("p (a b) -> p a b", a=2)[:, :, 127],
               ALU.subtract)
            ACTV(P, wsv, wsv[:, :], wsv, wsv[:, :], AF.Exp)
            P.dve(lambda e: e.memset(mqTz[:, :, :], 0.0), w=[mqTz])
            mz4 = mqTz.ap.rearrange("p (a b) t -> p a b t", b=2)
            CP(P, "dve", mqTz, mz4[0:64, :, 0, :], mqT, mqT[0:64, :, :])
            CP(P, "dve", mqTz, mz4[64:128, :, 1, :], mqT, mqT[64:128, :, :])
            TT(P, "dve", qsTz, qsTz.ap.rearrange("p (a b) t -> p a b t", b=2), mqTz, mz4, ebp, bc(ebp[:, :, :], 2, 2), ALU.mult)
            TT(P, "dve", kw, kw.ap.rearrange("p (h d) -> p h d", h=6), mktok, mktok.ap.rearrange("p (h d) -> p h d", h=6),
               wsv, bc(wsv[:, :], 2, 64), ALU.mult)
            P.pos("ST")
            for h in range(6):
                b = ps[2] if h < 4 else ps[3]
                o_ = (h % 4) * 128
                p_, r0 = h // 2, (h % 2) * 64
                MM(P, b, b[:, o_:o_ + 128], mkT, mkT[:, p_, :], mqTz, mqTz[:, h, :])
            TT(P, "dve", PT, PT[:, 0:4, :], ps[2], ps[2][:, :].rearrange("p (a b) -> p a b", a=4), DTf, DTf[:, 0:4, :], ALU.mult)
            TT(P, "dve", PT, PT[:, 4:6, :], ps[3], ps[3][:, 0:256].rearrange("p (a b) -> p a b", a=2), DTf, DTf[:, 4:6, :], ALU.mult)
            P.pos("numden")
            nd = ps[0]
            for h in range(6):
                p_, r0 = h // 2, (h % 2) * 64
                MM(P, nd, nd[:, h * 65:(h + 1) * 65], PT, PT[:, h, :], v1, v1[:, h, :], start=True, stop=(tt == 0))
                if tt > 0:
                    MM(P, nd, nd[:, h * 65:(h + 1) * 65], qsTz, qsTz[:, h, :], Cb, Cb[:, p_, :], start=False, stop=True)
            nd3 = nd[:, 0:390].rearrange("p (h e) -> p h e", h=6)
            CP(P, "dve", sm2, sm2[:, 40:46], nd, nd3[:, :, 64])
            TS(P, "dve", sm2, sm2[:, 48:54], sm2, sm2[:, 40:46], -1.0, None, ALU.mult)
            TT(P, "dve", sm2, sm2[:, 48:54], sm2, sm2[:, 48:54], sm2, sm2[:, 40:46], ALU.max)
            TS(P, "dve", sm2, sm2[:, 48:54], sm2, sm2[:, 48:54], 1.0, None, ALU.max)
            P.dve(lambda e: e.reciprocal(out=sm2[:, 56:62], in_=sm2[:, 48:54]), r=[sm2], w=[sm2])
            TT(P, "dve", yb, yb[:, 384:768].rearrange("p (h d) -> p h d", h=6), nd, nd3[:, :, 0:64], sm2, bc(sm2[:, 56:62], 2, 64),
               ALU.mult)
            P.pos("state")
            su = ps[1]
            for p_ in range(3):
                for s_ in range(2):
                    h = 2 * p_ + s_
                    MM(P, su, su[:, h * 65:(h + 1) * 65], kw, kw[:, p_ * 128:(p_ + 1) * 128], v1, v1[:, h, :])
            for p_ in range(3):
                for s_ in range(2):
                    h = 2 * p_ + s_
                    r0 = s_ * 64
                    STT(P, Cn, Cn[r0:r0 + 64, p_, :], Cn, Cn[r0:r0 + 64, p_, :], dec[r0:r0 + 64, p_:p_ + 1], su,
                        su[r0:r0 + 64, h * 65:(h + 1) * 65], ALU.mult, ALU.add, r=[dec])
            CP(P, "dve", Cb, Cb[:, :, :], Cn, Cn[:, :, :])
            P.pos("sg")
            u4 = uv.ap[:, 0:256].rearrange("p (g d) -> p g d", g=4)
            v4_ = uv.ap[:, 256:512].rearrange("p (g d) -> p g d", g=4)
            z4 = zq.ap[:, 0:256].rearrange("p (g d) -> p g d", g=4)
            TT(P, "dve", zq, z4, uv, v4_, uv, v4_, ALU.mult)
            RED(P, sm2, sm2[:, 0:4], zq, z4, ALU.add)
            rstd_of(sm2, sm2[:, 0:4], 1.0 / 64, sm2, sm2[:, 16:20], sm2, sm2[:, 32:36])
            TT(P, "dve", uv, v4_, uv, v4_, sm2, bc(sm2[:, 16:20], 2, 64), ALU.mult)
            TT(P, "dve", sgvb, sgvb[:, :], uv, uv[:, 256:512], svt, svt[:, 128:384], ALU.mult)
            gb = ps[5]
            for g in range(4):
                MM(P, gb, gb[:, 256 + g * 64:256 + (g + 1) * 64], sgw, sgw[:, g, :], sgvb, sgvb[:, g * 64:(g + 1) * 64])
            y4 = yb.ap[:, 768:1024].rearrange("p (g d) -> p g d", g=4)
            TT(P, "dve", yb, y4, gb, gb[:, 256:512].rearrange("p (g d) -> p g d", g=4), sgb, bc(sgb[:, :], 2, 64), ALU.add)
            TT(P, "dve", yb, y4, yb, y4, uv, u4, ALU.mult)
            P.pos("onorm")
            y10 = yb.ap[:, 384:1024].rearrange("p (h d) -> p h d", h=10)
            z10 = zq.ap[:, 0:640].rearrange("p (h d) -> p h d", h=10)
            TT(P, "dve", zq, z10, yb, y10, yb, y10, ALU.mult)
            RED(P, sm2, sm2[:, 0:10], zq, z10, ALU.add)
            rstd_of(sm2, sm2[:, 0:10], 1.0 / 64, sm2, sm2[:, 16:26], sm2, sm2[:, 32:42])
            TT(P, "dve", yb, y10, yb, y10, sm2, bc(sm2[:, 16:26], 2, 64), ALU.mult)
            TT(P, "dve", yb, yb[:, 384:1024], yb, yb[:, 384:1024], ogt_, ogt_[:, 384:1024], ALU.mult)
            TT(P, "dve", ymsb, ymsb[:, 0:384], yb, yb[:, 384:768], og, og[:, :], ALU.mult)
            CP(P, "act", ymsb, ymsb[:, 384:640], yb, yb[:, 768:1024])
            P.dma("pool", "ymsb", yms_d.ap()[tsl, :], ymsb[:, :], r=[ymsb], w=[P.dkey("yms", tt)])
        P.reset("abf", mb1)
        P.reset("af", mf1)
        P.pos("M2")
        mask4b = P.alloc("abf", (4, 512))
        y_bf = P.alloc("abf", (D,))
        yT = P.alloc("abf", (8, 128))
        qTg = P.alloc("abf", (3, QG))
        qTgz = P.alloc("abf", (6, QG))
        spb = [P.alloc("abf", (QG,)) for _ in range(2)]
        Lacc = [P.alloc("abf", (QG,)) for _ in range(2)]
        wT = [P.alloc("abf", (QG,)) for _ in range(2)]
        e32 = [P.alloc("af", (QG,)) for _ in range(2)]
        ysb = P.alloc("af", (TG, 384))
        sqb = P.alloc("af", (384,))
        yfs = P.alloc("af", (QG,))
        for r_ in range(4):
            P.dma("sp", "stg1", stg[1][:, 0:512], cst_d.ap()[:, C_MASK4 + r_ * 512:C_MASK4 + (r_ + 1) * 512], w=[stg[1]])
            CP(P, "dve", mask4b, mask4b[:, r_, :], stg[1], stg[1][:, 0:512])
        for g in range(S // QG):
            for i in range(TG):
                P.dma("sp", "qTg", qTg[:, :, i * 128:(i + 1) * 128], qT_d.ap()[g * TG + i].rearrange("p (a b) -> p a b", a=3),
                      r=[P.dkey("qT", g * TG + i)], w=[qTg])
            nkb = (g + 1) * TG
            P.dve(lambda e: e.memset(qTgz[:, :, :], 0.0), w=[qTgz])
            qz4 = qTgz.ap.rearrange("p (a b) t -> p a b t", b=2)
            CP(P, "dve", qTgz, qz4[0:64, :, 0, :], qTg, qTg[0:64, :, :])
            CP(P, "dve", qTgz, qz4[64:128, :, 1, :], qTg, qTg[64:128, :, :])
            for h in range(6):
                p_, r0 = h // 2, (h % 2) * 64
                ya = ps[4 + h % 2]
                pump(max(1, 120 // (6 * (S // QG))))
                it = 0
                for kb in range(nkb - 1, -1, -1):
                    rr = kb - g * TG
                    first = (kb == nkb - 1)
                    zb = ps[it % 2]
                    Bb = ps[2 + it % 2]
                    kap = kTa[:, p_, kb * 128:(kb + 1) * 128]
                    qap = qTgz[:, h, :]
                    MM(P, zb, zb[:, 0:QG], kTa, kap, qTgz, qap)
                    e_ = e32[it % 2]
                    s_ = spb[it % 2]
                    w_ = wT[it % 2]
                    ACTV(P, e_, e_[:, :], zb, zb[:, 0:QG], AF.Exp)
                    ACTV(P, s_, s_[:, :], e_, e_[:, :], AF.Ln, bias=one_t[:, 0:1], r=[one_t])
                    if rr >= 0:
                        TT(P, "dve", s_, s_[:, :], s_, s_[:, :], mask4b, mask4b[:, rr, 0:QG], ALU.mult)
                    MM(P, Bb, Bb[:, 0:QG], kTa, kap, qTgz, qap, start=True, stop=False)
                    MM(P, Bb, Bb[:, 0:QG], ntrib, ntrib[:, :], s_, s_[:, :], start=False, stop=first)
                    if not first:
                        MM(P, Bb, Bb[:, 0:QG], noneb, noneb[:, :], Lacc[it % 2], Lacc[it % 2][:, :], start=False, stop=True)
                    ACTV(P, w_, w_[:, :], Bb, Bb[:, 0:QG], AF.Exp)
                    if rr >= 0:
                        TT(P, "dve", w_, w_[:, :], w_, w_[:, :], mask4b, mask4b[:, rr, 0:QG], ALU.mult)
                    if kb > 0:
                        if first:
                            CP(P, "dve", Lacc[(it + 1) % 2], Lacc[(it + 1) % 2][:, :], s_, s_[:, :])
                        else:
                            TT(P, "dve", Lacc[(it + 1) % 2], Lacc[(it + 1) % 2][:, :], Lacc[it % 2], Lacc[it % 2][:, :], s_, s_[:, :],
                               ALU.add)
                    MM(P, ya, ya[:, 0:QG], va, va[:, kb, p_ * 128:(p_ + 1) * 128], w_, w_[:, :], start=first, stop=(kb == 0))
                    it += 1
                CP(P, "act", yfs, yfs[:, :], ya, ya[:, 0:QG])
                tb_ = ps[6]
                for qt in range(TG):
                    TR(P, tb_, tb_[:, qt * 128:(qt + 1) * 128], yfs, yfs[:, qt * 128:(qt + 1) * 128], identf, identf[:, :])
                CP(P, "act", ysb, ysb.ap.rearrange("p t (h d) -> p t h d", h=6)[:, :, h, :], tb_,
                   tb_[:, 0:TG * 128].rearrange("p (t d) -> p t d", t=TG)[:, :, r0:r0 + 64])
            for qt in range(TG):
                tt = g * TG + qt
                tsl = slice(tt * 128, (tt + 1) * 128)
                y6_ = ysb.ap[:, qt, :].rearrange("p (h d) -> p h d", h=6)
                s6_ = sqb.ap.rearrange("p (h d) -> p h d", h=6)
                TT(P, "dve", sqb, s6_, ysb, y6_, ysb, y6_, ALU.mult)
                RED(P, sm2, sm2[:, 0:6], sqb, s6_, ALU.add)
                rstd_of(sm2, sm2[:, 0:6], 1.0 / 64, sm2, sm2[:, 16:22], sm2, sm2[:, 32:38])
                TT(P, "dve", sqb, s6_, ysb, y6_, sm2, bc(sm2[:, 16:22], 2, 64), ALU.mult)
                TT(P, "dve", y_bf, y_bf[:, 0:384], sqb, sqb[:, :], ogt_, ogt_[:, 0:384], ALU.mult)
                P.dma("sp", "ybf", y_bf[:, 384:1024], yms_d.ap()[tsl, :], r=[P.dkey("yms", tt)], w=[y_bf])
                for kc in range(8):
                    TR(P, psT, psT[:, kc * 128:(kc + 1) * 128], y_bf, y_bf[:, kc * 128:(kc + 1) * 128], identb, identb[:, :])
                CP(P, "act", yT, yT[:, :, :], psT, psT[:, :].rearrange("p (a b) -> p a b", a=8))
                P.dma("sp", "xt", xt[:, :], out_d.ap()[tsl, :], r=[dx(tt)], w=[xt])
                for hf in range(2):
                    ob = ps[6] if hf == 0 else ps[0]
                    for kc in range(8):
                        MM(P, ob, ob[:, :], yT, yT[:, kc, :], wout, wout[:, kc, hf * 512:(hf + 1) * 512], start=(kc == 0), stop=(kc == 7))
                    TT(P, "dve", xt, xt[:, hf * 512:(hf + 1) * 512], xt, xt[:, hf * 512:(hf + 1) * 512], ob, ob[:, :], ALU.add)
                P.dma("pool", "stxm", out_d.ap()[tsl, :], xt[:, :], r=[xt], w=[dx(tt)])
        P.reset("abf", mb)
        P.reset("af", mf)

    for l in range(L):
        cg = ffn_cast_gen(l) if "ffn" in phases else None
        if "mix" in phases:
            mix_layer(l, cg)
        if "ffn" in phases:
            for _ in cg:
                pass
            ffn_layer(l)

    P.finalize()
    return nc, es, P


def prep_weights(inp, L, ffn=True):
    f = lambda a: np.ascontiguousarray(np.asarray(a, dtype=np.float32))
    w = {}
    w["cst"] = make_consts()
    w["gvec"] = f(np.stack([inp["norm1_g"][:L], inp["norm2_g"][:L], inp["out_g"][:L]], axis=1))
    w["svec"] = f(np.concatenate([inp["sb_qn_g"][:L], inp["sb_kn_g"][:L], inp["sg_vn_g"][:L], inp["ml_i_b"][:L],
                                  inp["ml_f_b"][:L]], axis=1))
    cw = np.asarray(inp["ml_conv_w"][:L]).reshape(L, 4, 6, 128).transpose(0, 3, 2, 1)
    cb = np.asarray(inp["ml_conv_b"][:L]).reshape(L, 6, 128).transpose(0, 2, 1)[..., None]
    w["convw"] = f(np.concatenate([cw, cb], axis=3).reshape(L, 128, 30))
    w["sgw"] = f(np.asarray(inp["sg_w"][:L]).transpose(0, 3, 1, 2).reshape(L, 128, 512))
    w["sgb"] = f(np.asarray(inp["sg_b"][:L]).transpose(0, 2, 1))
    win = np.asarray(inp["w_in"][:L])
    cols_a = np.concatenate([np.arange(0, 1152), np.arange(1920, 2688), np.arange(2700, 3212), np.arange(2688, 2700)])
    cols_b = np.arange(1152, 1920)

    def kmaj(m):
        n = m.shape[2]
        return f(m.reshape(L, 8, 128, n).transpose(0, 2, 1, 3).reshape(L, 128, 8 * n))
    w["wina"] = kmaj(win[:, :, cols_a])
    w["winb"] = kmaj(win[:, :, cols_b])
    w["wout"] = kmaj(np.asarray(inp["w_out"][:L]))
    w["wq"] = kmaj(np.asarray(inp["peer_wq"][:L]))
    w["pkeys"] = f(np.asarray(inp["peer_keys"][:L]).reshape(L, 16, 128, 128).transpose(0, 3, 1, 2).reshape(L, 128, 2048))
    if not ffn:
        return w
    u = np.asarray(inp["peer_u"][:L]).reshape(L, 128, 128, 8, 128)
    w["ut"] = f(u.transpose(0, 2, 4, 3, 1).reshape(L, 128, 128, 1024))
    v = np.asarray(inp["peer_v"][:L]).reshape(L, 128, 128, 1024)
    w["vt"] = f(v.transpose(0, 2, 1, 3))
    return w


_CACHE = {}


def run(inputs, S, L, phases=("mix", "ffn"), ncores=NCORES):
    x = np.asarray(inputs["x"], dtype=np.float32)
    key = (S, L, tuple(phases))
    if key not in _CACHE:
        _CACHE[key] = build(S, L, phases)
    nc, es, P = _CACHE[key]
    w = prep_weights(inputs, L, "ffn" in phases)
    in_maps = []
    for c in range(ncores):
        m = dict(w)
        m["x"] = np.ascontiguousarray(x[c, :S])
        in_maps.append(m)
    res = run_bass_kernel_spmd(nc, in_maps, core_ids=list(range(ncores)))
    return np.stack([np.asarray(r["out"]) for r in res.results], axis=0)


def kernel(**inputs):
    return run(inputs, 4096, 4).astype(np.float32)
```

```python
from contextlib import ExitStack
import numpy as np
import ml_dtypes
import concourse.bass as bass
import concourse.mybir as mybir
from concourse.ap import AP
from concourse.bass_utils import run_bass_kernel_spmd

F32 = mybir.dt.float32
BF16 = mybir.dt.bfloat16
U32 = mybir.dt.uint32
ALU = mybir.AluOpType
AF = mybir.ActivationFunctionType
AX = mybir.AxisListType

D = 1024
NCORES = 8
EPS = 1e-6
NEG = -1.0e30
COLS_A = 2444
OQ, OK_, OV, OMV, OMO, OSG, OMI, OMF = 0, 384, 768, 1152, 1536, 1920, 2432, 2438


class Tile:
    __slots__ = ("ap", "keys")

    def __init__(self, ap, keys):
        self.ap = ap
        self.keys = tuple(keys)

    def __getitem__(self, k):
        return self.ap[k]


class Prog:
    ENGS = ("pe", "act", "dve", "pool", "sp")
    PAGE = 512

    def __init__(self, nc, es):
        self.nc = nc
        self.es = es
        self.ops = []
        self.nkey = 0
        self.arenas = {}
        self.dma_keys = {}

    def key(self):
        self.nkey += 1
        return ("k", self.nkey)

    def static(self, name, shape, dtype):
        t = self.es.enter_context(self.nc.sbuf_tensor("sb_" + name, list(shape), dtype))
        return Tile(t[:], [self.key()])

    def psum(self, name, shape, dtype):
        t = self.es.enter_context(self.nc.psum_tensor(name, list(shape), dtype))
        return Tile(t[:], [self.key()])

    def arena(self, name, nelem, dtype):
        t = self.es.enter_context(self.nc.sbuf_tensor(name, [128, nelem], dtype))
        self.arenas[name] = dict(t=t, n=nelem, off=0)

    def mark(self, name):
        return self.arenas["abf"]["off"]

    def reset(self, name, m):
        self.arenas["abf"]["off"] = m

    def alloc(self, name, shape):
        f32 = (name == "af")
        name = "abf"
        a = self.arenas[name]
        n = int(np.prod(shape))
        nb = 2 * n if f32 else n
        off = a["off"]
        npg = (nb + self.PAGE - 1) // self.PAGE
        assert off + npg * self.PAGE <= a["n"], f"arena {name} overflow need {off + npg * self.PAGE} have {a['n']}"
        a["off"] = off + npg * self.PAGE
        ap = a["t"][:, off:off + nb]
        if f32:
            ap = ap.bitcast(F32)
        if len(shape) == 2:
            ap = ap.rearrange("p (a b) -> p a b", a=shape[0])
        elif len(shape) == 3:
            ap = ap.rearrange("p (a b c) -> p a b c", a=shape[0], b=shape[1])
        keys = [(name, off // self.PAGE + i) for i in range(npg)]
        return Tile(ap, keys)

    def dkey(self, *k):
        return Tile(None, [("d",) + tuple(k)])

    def add(self, eng, fn, r=(), w=(), dma=None):
        import os
        if len(self.ops) >= int(os.environ.get("OPLIMIT", "100000000")):
            return
        rk = []
        for t in r:
            rk.extend(t.keys)
        wk = []
        for t in w:
            wk.extend(t.keys)
        self.ops.append([eng, fn, rk, wk, dma])

    def pos(self, name):
        import os
        if os.environ.get("SHOWPOS"):
            print("POS", name, len(self.ops), flush=True)

    def pe(self, fn, r=(), w=()):
        self.add("pe", fn, r, w)

    def act(self, fn, r=(), w=()):
        self.add("act", fn, r, w)

    def dve(self, fn, r=(), w=()):
        self.add("dve", fn, r, w)

    def pool(self, fn, r=(), w=()):
        self.add("pool", fn, r, w)

    def dma(self, q, key, out, in_, r=(), w=()):
        self.add(q, lambda e: e.dma_start(out=out, in_=in_), r, w, dma=key)

    def finalize(self):
        nc = self.nc
        ops = self.ops
        n = len(ops)
        lastw = {}
        readers = {}
        deps_all = [None] * n
        for k, (eng, fn, rk, wk, dma) in enumerate(ops):
            deps = set()
            for b in rk:
                d = lastw.get(b)
                if d is not None:
                    deps.add(d)
            for b in wk:
                d = lastw.get(b)
                if d is not None:
                    deps.add(d)
                rd = readers.get(b)
                if rd:
                    deps.update(rd)
            deps.discard(k)
            deps_all[k] = deps
            for b in rk:
                readers.setdefault(b, []).append(k)
            for b in wk:
                lastw[b] = k
                readers[b] = []
        waited = {e: {} for e in self.ENGS}
        need = [None] * n
        signal = [False] * n
        for k in range(n):
            eng = ops[k][0]
            wl = waited[eng]
            nd = {}
            for d in deps_all[k]:
                deng, _, _, _, ddma = ops[d]
                if ddma is not None:
                    pk = ("dma", ddma)
                    if wl.get(pk, -1) >= d:
                        continue
                    nd[pk] = max(nd.get(pk, -1), d)
                else:
                    if deng == "pe" and eng == "pe":
                        continue
                    pk = ("eng", deng)
                    if wl.get(pk, -1) >= d:
                        continue
                    nd[pk] = max(nd.get(pk, -1), d)
            for pk, d in nd.items():
                wl[pk] = max(wl.get(pk, -1), d)
                if pk[0] == "eng":
                    signal[d] = True
            need[k] = nd
        sem_eng = {e: self.es.enter_context(nc.semaphore("s_" + e)) for e in self.ENGS}
        cnt = {e: 0 for e in self.ENGS}
        val = [0] * n
        dma_cnt = {}
        dma_idx = {}
        for k in range(n):
            eng, _, _, _, dma = ops[k]
            if dma is not None:
                dma_cnt[dma] = dma_cnt.get(dma, 0) + 1
                dma_idx.setdefault(dma, []).append(k)
                val[k] = 16 * dma_cnt[dma]
            elif signal[k]:
                cnt[eng] += 1
                val[k] = cnt[eng]
        sem_dma = {dk: self.es.enter_context(nc.semaphore("d_%d" % i)) for i, dk in enumerate(dma_cnt)}
        import bisect
        per = {e: [] for e in self.ENGS}
        for k in range(n):
            per[ops[k][0]].append(k)
        final_waits = [(sem_dma[dk], 16 * c) for dk, c in dma_cnt.items()]
        self.stats = {e: len(per[e]) for e in self.ENGS}
        self.stats["signals"] = sum(signal)

        def emit(e, name):
            for k in per[name]:
                _, fn, _, _, dma = ops[k]
                for pk, d in need[k].items():
                    if pk[0] == "eng":
                        e.wait_ge(sem_eng[pk[1]], val[d])
                    else:
                        lst = dma_idx[pk[1]]
                        c = bisect.bisect_left(lst, k)
                        e.wait_ge(sem_dma[pk[1]], 16 * c)
                ins = fn(e)
                if dma is not None:
                    ins.then_inc(sem_dma[dma], 16)
                elif signal[k]:
                    ins.then_inc(sem_eng[name], 1)
            if name == "pool":
                for s, v in final_waits:
                    e.wait_ge(s, v)
                for en in self.ENGS:
                    if en != "pool" and cnt[en] > 0:
                        e.wait_ge(sem_eng[en], cnt[en])

        block = self.es.enter_context(nc.Block())

        @block.sync
        def _(e):
            emit(e, "sp")

        @block.tensor
        def _(e):
            emit(e, "pe")

        @block.scalar
        def _(e):
            emit(e, "act")

        @block.vector
        def _(e):
            emit(e, "dve")

        @block.gpsimd
        def _(e):
            emit(e, "pool")


def bcast_rows(dram_ap_1d_tensor, offset, n):
    return AP(dram_ap_1d_tensor, offset, [[0, 128], [1, n]])


def MM(P, ot, oap, lt, lap, rt, rap, start=True, stop=True):
    P.pe(lambda e: e.matmul(oap, lhsT=lap, rhs=rap, start=start, stop=stop), r=[lt, rt], w=[ot])


def TR(P, ot, oap, it, iap, idt, idap):
    P.pe(lambda e: e.transpose(oap, iap, idap), r=[it, idt], w=[ot])


def ACTV(P, ot, oap, it, iap, func, bias=0.0, scale=1.0, r=(), accum=None, w=()):
    if accum is None:
        P.act(lambda e: e.activation(out=oap, in_=iap, func=func, bias=bias, scale=scale), r=[it] + list(r), w=[ot] + list(w))
    else:
        P.act(lambda e: e.activation(out=oap, in_=iap, func=func, bias=bias, scale=scale, accum_out=accum),
              r=[it] + list(r), w=[ot] + list(w))


def TT(P, eng, ot, oap, at, aap, bt, bap, op):
    P.add(eng, lambda e: e.tensor_tensor(out=oap, in0=aap, in1=bap, op=op), r=[at, bt], w=[ot])


def TS(P, eng, ot, oap, at, aap, s1, s2, op0, op1=None, r=()):
    if op1 is None:
        P.add(eng, lambda e: e.tensor_scalar(out=oap, in0=aap, scalar1=s1, scalar2=None, op0=op0), r=[at] + list(r), w=[ot])
    else:
        P.add(eng, lambda e: e.tensor_scalar(out=oap, in0=aap, scalar1=s1, scalar2=s2, op0=op0, op1=op1),
              r=[at] + list(r), w=[ot])


def STT(P, ot, oap, at, aap, sc, bt, bap, op0, op1, r=()):
    P.dve(lambda e: e.scalar_tensor_tensor(out=oap, in0=aap, scalar=sc, in1=bap, op0=op0, op1=op1),
          r=[at, bt] + list(r), w=[ot])


def CP(P, eng, ot, oap, it, iap):
    if eng == "act":
        P.act(lambda e: e.copy(out=oap, in_=iap), r=[it], w=[ot])
    else:
        P.add(eng, lambda e: e.tensor_copy(out=oap, in_=iap), r=[it], w=[ot])


def RED(P, ot, oap, it, iap, op, axis=None):
    axis = AX.X if axis is None else axis
    P.dve(lambda e: e.tensor_reduce(out=oap, in_=iap, axis=axis, op=op), r=[it], w=[ot])


def bc(ap, axis, n):
    a = ap.unsqueeze(axis)
    shp = list(a.shape)
    shp[axis] = n
    return a.broadcast_to(shp)


C_IDF, C_IOTA, C_IOTA16, C_THR, C_TRI, C_NTRI, C_NONE, C_MASK4 = 0, 128, 256, 272, 288, 416, 544, 672
NCST = 672 + 2048


def make_consts():
    c = np.zeros((128, NCST), np.float32)
    c[:, C_IDF:C_IDF + 128] = np.eye(128, dtype=np.float32)
    c[:, C_IOTA:C_IOTA + 128] = np.arange(128, dtype=np.float32)[None, :]
    c[:, C_IOTA16:C_IOTA16 + 16] = np.arange(16, dtype=np.float32)[None, :]
    c[:, C_THR:C_THR + 16] = (16.0 * np.arange(1, 17, dtype=np.float32))[None, :]
    j = np.arange(128)[:, None]
    t = np.arange(128)[None, :]
    c[:, C_TRI:C_TRI + 128] = (j <= t).astype(np.float32)
    c[:, C_NTRI:C_NTRI + 128] = -(j >= t).astype(np.float32)
    c[:, C_NONE:C_NONE + 128] = -1.0
    q = np.arange(512)[None, :]
    for r in range(4):
        c[:, C_MASK4 + r * 512:C_MASK4 + (r + 1) * 512] = ((128 * r + j) < q).astype(np.float32)
    return c


def build(S, L, phases=("mix", "ffn"), dbg=False):
    NT = S // 128
    nc = bass.Bass("TRN2", target_bir_lowering=False)
    es = ExitStack()
    P = Prog(nc, es)

    def din(name, shape):
        return nc.dram_tensor(name, list(shape), F32, kind="ExternalInput")

    x_d = din("x", [S, D])
    cst_d = din("cst", [128, NCST])
    gvec_d = din("gvec", [L, 3, D])
    svec_d = din("svec", [L, 396])
    convw_d = din("convw", [L, 128, 30])
    sgw_d = din("sgw", [L, 128, 512])
    sgb_d = din("sgb", [L, 128, 4])
    wina_d = din("wina", [L, 128, 8 * COLS_A])
    winb_d = din("winb", [L, 128, 8 * 768])
    wout_d = din("wout", [L, 128, 8 * 1024])
    wq_d = din("wq", [L, 128, 8 * 2048])
    keys_d = din("pkeys", [L, 128, 16 * 128])
    if "ffn" in phases:
        ut_d = din("ut", [L, 128, 128, 1024])
        vt_d = din("vt", [L, 128, 128, 1024])
    out_d = nc.dram_tensor("out", [S, D], F32, kind="ExternalOutput")
    ubf_d = nc.dram_tensor("ubf", [L, 128, 128, 1024], BF16, kind="Internal")
    vbf_d = nc.dram_tensor("vbf", [L, 128, 128, 1024], BF16, kind="Internal")
    yms_d = nc.dram_tensor("yms", [S, 640], BF16, kind="Internal")
    dbg_d = nc.dram_tensor("dbg", [S, D], F32, kind="ExternalOutput") if dbg else None

    P.arena("abf", 168 * 512, BF16)

    ps = [P.psum("ps%d" % i, [128, 512], F32) for i in range(7)]
    psT = P.psum("psT", [128, 1024], BF16)
    psTf = Tile(psT.ap.bitcast(F32), psT.keys)

    cstf = P.static("cstf", [128, C_MASK4], F32)
    P.dma("sp", "cst", cstf[:, :], cst_d.ap()[:, 0:C_MASK4], w=[cstf])
    identb = P.static("identb", [128, 128], BF16)
    iotab = P.static("iotab", [128, 128], BF16)
    CP(P, "dve", identb, identb[:, :], cstf, cstf[:, C_IDF:C_IDF + 128])
    CP(P, "dve", iotab, iotab[:, :], cstf, cstf[:, C_IOTA:C_IOTA + 128])
    identf = Tile(cstf[:, C_IDF:C_IDF + 128], cstf.keys)

    svt = P.static("svt", [128, 396], F32)
    stg = [P.static("stg%d" % i, [128, 1024], F32) for i in range(2)]
    cbf = [P.static("cbf%d" % i, [128, 1024], BF16) for i in range(2)]
    cnt = {"stg": 0, "cast": 0}

    def dx(tt):
        return P.dkey("x", tt)

    for tt in range(NT):
        P.dma("sp", "xcp", out_d.ap()[tt * 128:(tt + 1) * 128, :], x_d.ap()[tt * 128:(tt + 1) * 128, :], w=[dx(tt)])

    def load_cast(dst_t, dst_ap2, src_ap2, n):
        for c0 in range(0, n, 1024):
            w_ = min(1024, n - c0)
            i = cnt["stg"] % 2
            cnt["stg"] += 1
            P.dma("sp", "stg%d" % i, stg[i][:, 0:w_], src_ap2[:, c0:c0 + w_], w=[stg[i]])
            eng = ("dve", "act")[cnt["cast"] % 2]
            cnt["cast"] += 1
            CP(P, eng, dst_t, dst_ap2[:, c0:c0 + w_], stg[i], stg[i][:, 0:w_])

    def rstd_of(ss_t, ss_ap, n_inv, out_t, out_ap, tmp_t, tmp_ap):
        TS(P, "dve", tmp_t, tmp_ap, ss_t, ss_ap, n_inv, EPS, ALU.mult, ALU.add)
        ACTV(P, tmp_t, tmp_ap, tmp_t, tmp_ap, AF.Ln)
        ACTV(P, out_t, out_ap, tmp_t, tmp_ap, AF.Exp, scale=-0.5)

    def norm_transpose(xt, gt, xn, xnT_t, xnT_ap3, ss, tmp):
        ACTV(P, xn, xn[:, :], xt, xt[:, :], AF.Square, accum=ss[:, 0:1], w=[ss])
        rstd_of(ss, ss[:, 0:1], 1.0 / D, ss, ss[:, 1:2], tmp, tmp[:, 0:1])
        STT(P, xn, xn[:, :], xt, xt[:, :], ss[:, 1:2], gt, gt[:, :], ALU.mult, ALU.mult, r=[ss])
        for kc in range(8):
            TR(P, psT, psT[:, kc * 128:(kc + 1) * 128], xn, xn[:, kc * 128:(kc + 1) * 128], identb, identb[:, :])
        CP(P, "act", xnT_t, xnT_ap3, psT, psT[:, :].rearrange("p (a b) -> p a b", a=8))


    ffn_st = (P.static("v16", [128, 16, 16], F32), P.static("i16u", [128, 16, 16], U32), P.static("i16f", [128, 16, 16], F32),
              P.static("c16", [128, 8, 16], F32), P.static("p16u", [128, 8, 16], U32), P.static("p16f", [128, 8, 16], F32),
              P.static("kq", [128, 8, 16], F32), P.static("kr", [128, 8, 16], F32), P.static("isel", [128, 8, 16], F32),
              P.static("jsel", [128, 8, 16], F32), P.static("gsel", [128, 8, 16], F32), P.static("smf", [128, 32], F32),
              P.static("fss", [128, 4], F32))

    wqbf_d = nc.dram_tensor("wqbf", [L, 128, 8 * 2048], BF16, kind="Internal") if "ffn" in phases else None

    def ffn_cast_gen(l):
        def one(src_ap, dst_ap, n, dk):
            i = cnt["stg"] % 2
            cnt["stg"] += 1
            P.dma("sp", "stg%d" % i, stg[i][:, 0:n], src_ap, w=[stg[i]])
            eng = ("dve", "act")[cnt["cast"] % 2]
            cnt["cast"] += 1
            CP(P, eng, cbf[i], cbf[i][:, 0:n], stg[i], stg[i][:, 0:n])
            P.dma("pool", "cbf%d" % i, dst_ap, cbf[i][:, 0:n], r=[cbf[i]], w=[dk])
        for c in range(16):
            one(wq_d.ap()[l][:, c * 1024:(c + 1) * 1024], wqbf_d.ap()[l][:, c * 1024:(c + 1) * 1024], 1024, P.dkey("wqbf", l))
            yield
        for j in range(128):
            one(ut_d.ap()[l, j], ubf_d.ap()[l, j], 1024, P.dkey("ubf", l, j))
            yield
            one(vt_d.ap()[l, j], vbf_d.ap()[l, j], 1024, P.dkey("vbf", l, j))
            yield

    def ffn_layer(l):
        mb, mf = P.mark("abf"), P.mark("af")
        TB = 256 if S >= 256 else 128
        NTB = TB // 128
        NBLK = S // TB
        (v16, i16u, i16f, c16, p16u, p16f, kq, kr, isel, jsel, gsel, sm, ss) = ffn_st
        keys_sb = P.alloc("abf", (16, 128))
        xn = P.alloc("abf", (D,))
        xnT2 = [P.alloc("abf", (8, TB)) for _ in range(2)]
        qT = P.alloc("abf", (16, TB))
        selT2 = [P.alloc("abf", (3, TB)) for _ in range(2)]
        OIg = P.alloc("abf", (16, 128))
        OJ = P.alloc("abf", (16, 128))
        W_sb = P.alloc("abf", (TB, 128))
        NR = 4
        u_r = [P.alloc("abf", (8, 128)) for _ in range(NR)]
        v_r = [P.alloc("abf", (D,)) for _ in range(NR)]
        NH = 3
        Hh = [P.alloc("abf", (TB,)) for _ in range(NH)]
        npb = 512 // TB
        wqs = [P.alloc("abf", (8, npb * 128)) for _ in range(2)]
        xres2 = [[P.alloc("af", (D,)) for _ in range(NTB)] for _ in range(2)]
        sc = P.alloc("af", (16, 128))
        sc2 = P.alloc("af", (16, 128))
        Gs = [P.alloc("af", (TB,)) for _ in range(NH)]
        g2t = P.alloc("af", (D,))
        cand = Tile(sc.ap.rearrange("p a b -> p (a b)").rearrange("p (h x) -> p h x", h=8), sc.keys)
        cand2 = Tile(sc2.ap.rearrange("p a b -> p (a b)").rearrange("p (h x) -> p h x", h=8), sc2.keys)
        oh = Tile(sc2.ap.rearrange("p a b -> p (a b)").rearrange("p (h r k) -> p h r k", h=8, r=16), sc2.keys)

        P.dma("sp", "g2t", g2t[:, :], bcast_rows(gvec_d, (l * 3 + 1) * D, D), w=[g2t])
        load_cast(keys_sb, keys_sb.ap.rearrange("p a b -> p (a b)"), keys_d.ap()[l], 16 * 128)

        iota3 = AP(iotab.ap.tensor, iotab.ap.offset, [list(iotab.ap.ap[0]), [0, 16], [1, 128]])
        iota16_4 = AP(cstf.ap.tensor, cstf.ap.offset + C_IOTA16, [list(cstf.ap.ap[0]), [0, 8], [0, 16], [1, 16]])
        thr_4 = AP(cstf.ap.tensor, cstf.ap.offset + C_THR, [list(cstf.ap.ap[0]), [0, 8], [0, 16], [1, 16]])
        misc = [ps[6], psTf]
        mcnt = [0]
        wcnt = [0]
        wq3 = wqbf_d.ap()[l].rearrange("p (k c) -> p k c", k=8)

        def mbank():
            b = misc[mcnt[0] % 2]
            mcnt[0] += 1
            return b

        def f_sel(blk):
            par = blk % 2
            xres, xnT, selT = xres2[par], xnT2[par], selT2[par]
            for ti in range(NTB):
                tt = blk * NTB + ti
                P.dma("sp", "xres%d_%d" % (par, ti), xres[ti][:, :], out_d.ap()[tt * 128:(tt + 1) * 128, :], r=[dx(tt)], w=[xres[ti]])
                norm_transpose(xres[ti], g2t, xn, xnT, xnT[:, :, ti * 128:(ti + 1) * 128], ss, sm)
                yield
            for h0 in range(0, 16, npb):
                ws = wqs[wcnt[0] % 2]
                P.dma("sp", "wqs%d" % (wcnt[0] % 2), ws[:, :, :], wq3[:, :, h0 * 128:(h0 + npb) * 128], r=[P.dkey("wqbf", l)], w=[ws])
                wcnt[0] += 1
                b = mbank()
                for s_ in range(npb):
                    for kc in range(8):
                        MM(P, b, b[:, s_ * TB:(s_ + 1) * TB], ws, ws[:, kc, s_ * 128:(s_ + 1) * 128],
                           xnT, xnT[:, kc, :], start=(kc == 0), stop=(kc == 7))
                CP(P, "act", qT, qT[:, h0:h0 + npb, :], b, b[:, :].rearrange("p (a b) -> p a b", a=npb))
                yield
            for ti in range(NTB):
                tsl = slice(ti * 128, (ti + 1) * 128)
                for q4 in range(4):
                    b = mbank()
                    for s4 in range(4):
                        hc = 4 * q4 + s4
                        MM(P, b, b[:, s4 * 128:(s4 + 1) * 128], qT, qT[:, hc, tsl], keys_sb, keys_sb[:, hc, :])
                    CP(P, "act", sc, sc[:, 4 * q4:4 * q4 + 4, :], b, b[:, :].rearrange("p (a b) -> p a b", a=4))
                    yield
                for hc in range(16):
                    P.dve(lambda e, hc=hc: e.max(out=v16[:, hc, 0:8], in_=sc[:, hc, :]), r=[sc], w=[v16])
                    P.dve(lambda e, hc=hc: e.max_index(out=i16u[:, hc, 0:8], in_max=v16[:, hc, 0:8], in_values=sc[:, hc, :]),
                          r=[sc, v16], w=[i16u])
                    P.dve(lambda e, hc=hc: e.match_replace(out=sc2[:, hc, :], in_to_replace=v16[:, hc, 0:8],
                                                           in_values=sc[:, hc, :], imm_value=NEG), r=[sc, v16], w=[sc2])
                    P.dve(lambda e, hc=hc: e.max(out=v16[:, hc, 8:16], in_=sc2[:, hc, :]), r=[sc2], w=[v16])
                    P.dve(lambda e, hc=hc: e.max_index(out=i16u[:, hc, 8:16], in_max=v16[:, hc, 8:16], in_values=sc2[:, hc, :]),
                          r=[sc2, v16], w=[i16u])
                    yield
                CP(P, "dve", i16f, i16f[:, :, :], i16u, i16u[:, :, :])
                v4 = v16.ap.rearrange("p (h c) k -> p h c k", c=2)
                i4 = i16f.ap.rearrange("p (h c) k -> p h c k", c=2)
                TT(P, "dve", cand, cand.ap.rearrange("p h (a b) -> p h a b", a=16), v16, bc(v4[:, :, 0, :], 3, 16),
                   v16, bc(v4[:, :, 1, :], 2, 16), ALU.add)
                for h in range(8):
                    P.dve(lambda e, h=h: e.max(out=c16[:, h, 0:8], in_=cand[:, h, :]), r=[cand], w=[c16])
                    P.dve(lambda e, h=h: e.max_index(out=p16u[:, h, 0:8], in_max=c16[:, h, 0:8], in_values=cand[:, h, :]),
                          r=[cand, c16], w=[p16u])
                    P.dve(lambda e, h=h: e.match_replace(out=cand2[:, h, :], in_to_replace=c16[:, h, 0:8],
                                                         in_values=cand[:, h, :], imm_value=NEG), r=[cand, c16], w=[cand2])
                    P.dve(lambda e, h=h: e.max(out=c16[:, h, 8:16], in_=cand2[:, h, :]), r=[cand2], w=[c16])
                    P.dve(lambda e, h=h: e.max_index(out=p16u[:, h, 8:16], in_max=c16[:, h, 8:16], in_values=cand2[:, h, :]),
                          r=[cand2, c16], w=[p16u])
                    yield
                CP(P, "dve", p16f, p16f[:, :, :], p16u, p16u[:, :, :])
                TT(P, "dve", oh, oh[:, :, :, :], p16f, bc(p16f[:, :, :], 3, 16), cstf, thr_4, ALU.is_ge)
                RED(P, kq, kq[:, :, :], oh, oh[:, :, :, :], ALU.add)
                STT(P, kr, kr[:, :, :], kq, kq[:, :, :], -16.0, p16f, p16f[:, :, :], ALU.mult, ALU.add)
                yield
                for (kk, src, dst) in ((kq, i4[:, :, 0, :], isel), (kr, i4[:, :, 1, :], jsel)):
                    TT(P, "dve", oh, oh[:, :, :, :], kk, bc(kk[:, :, :], 3, 16), cstf, iota16_4, ALU.is_equal)
                    TT(P, "dve", oh, oh[:, :, :, :], oh, oh[:, :, :, :], i16f, bc(src, 2, 16), ALU.mult)
                    RED(P, dst, dst[:, :, :], oh, oh[:, :, :, :], ALU.add)
                    yield
                RED(P, sm, sm[:, 0:8], c16, c16[:, :, :], ALU.max)
                TT(P, "dve", gsel, gsel[:, :, :], c16, c16[:, :, :], sm, bc(sm[:, 0:8], 2, 16), ALU.subtract)
                ACTV(P, gsel, gsel[:, :, :], gsel, gsel[:, :, :], AF.Exp)
                RED(P, sm, sm[:, 8:16], gsel, gsel[:, :, :], ALU.add)
                P.dve(lambda e: e.reciprocal(out=sm[:, 16:24], in_=sm[:, 8:16]), r=[sm], w=[sm])
                TT(P, "dve", gsel, gsel[:, :, :], gsel, gsel[:, :, :], sm, bc(sm[:, 16:24], 2, 16), ALU.mult)
                yield
                b = mbank()
                for q_, src in enumerate((isel, jsel, gsel)):
                    TR(P, b, b[:, q_ * 128:(q_ + 1) * 128], src, src.ap.rearrange("p h r -> p (h r)"), identf, identf[:, :])
                CP(P, "act", selT, selT[:, :, tsl], b, b[:, 0:384].rearrange("p (a b) -> p a b", a=3))
                yield

        def f_w(blk):
            selT = selT2[blk % 2]
            for tb in range(TB // 16):
                t0 = tb * 16
                TT(P, "dve", OIg, OIg[:, :, :], iotab, iota3, selT, bc(selT[:, 0, t0:t0 + 16], 2, 128), ALU.is_equal)
                TT(P, "pool", OIg, OIg[:, :, :], OIg, OIg[:, :, :], selT, bc(selT[:, 2, t0:t0 + 16], 2, 128), ALU.mult)
                TT(P, "dve", OJ, OJ[:, :, :], iotab, iota3, selT, bc(selT[:, 1, t0:t0 + 16], 2, 128), ALU.is_equal)
                for q_ in range(4):
                    b = mbank()
                    for u_ in range(4):
                        t = q_ * 4 + u_
                        MM(P, b, b[:, u_ * 128:(u_ + 1) * 128], OIg, OIg[:, t, :], OJ, OJ[:, t, :])
                    CP(P, ("act", "dve")[q_ % 2], W_sb, W_sb[:, t0 + q_ * 4:t0 + q_ * 4 + 4, :], b,
                       b[:, :].rearrange("p (a b) -> p a b", a=4))

        def f_main(blk, gen):
            xnT = xnT2[blk % 2]

            def issue_a(j):
                sl = j % NR
                P.dma("sp", "u%d" % sl, u_r[sl].ap.rearrange("p a b -> p (a b)"), ubf_d.ap()[l, j],
                      r=[P.dkey("ubf", l, j)], w=[u_r[sl]])
                P.dma("sp", "v%d" % sl, v_r[sl][:, :], vbf_d.ap()[l, j], r=[P.dkey("vbf", l, j)], w=[v_r[sl]])
                ab = ps[4 + (j % 2)]
                for kc in range(8):
                    MM(P, ab, ab[:, 0:TB], u_r[sl], u_r[sl][:, kc, :], xnT, xnT[:, kc, :], start=(kc == 0), stop=(kc == 7))

            issue_a(0)
            for j in range(128):
                sl = j % NR
                ab = ps[4 + (j % 2)]
                g_ = Gs[j % NH]
                h_ = Hh[j % NH]
                ACTV(P, g_, g_[:, :], ab, ab[:, 0:TB], AF.Gelu_apprx_tanh)
                TT(P, "dve", h_, h_[:, :], g_, g_[:, :], W_sb, W_sb[:, :, j], ALU.mult)
                if j + 1 < 128:
                    issue_a(j + 1)
                for ti in range(NTB):
                    for hf in range(2):
                        yb = ps[ti * 2 + hf]
                        MM(P, yb, yb[:, :], h_, h_[:, ti * 128:(ti + 1) * 128], v_r[sl], v_r[sl][:, hf * 512:(hf + 1) * 512],
                           start=(j == 0), stop=(j == 127))
                if gen is not None and j >= 4:
                    next(gen, None)
            if gen is not None:
                for _ in gen:
                    pass

        def f_out(blk):
            xres = xres2[blk % 2]
            for ti in range(NTB):
                tt = blk * NTB + ti
                for hf in range(2):
                    yb = ps[ti * 2 + hf]
                    TT(P, "dve", xres[ti], xres[ti][:, hf * 512:(hf + 1) * 512], xres[ti], xres[ti][:, hf * 512:(hf + 1) * 512],
                       yb, yb[:, :], ALU.add)
                P.dma("pool", "stx%d_%d" % (blk % 2, ti), out_d.ap()[tt * 128:(tt + 1) * 128, :], xres[ti][:, :], r=[xres[ti]], w=[dx(tt)])

        for _ in f_sel(0):
            pass
        for blk in range(NBLK):
            f_w(blk)
            f_main(blk, f_sel(blk + 1) if blk + 1 < NBLK else None)
            f_out(blk)
        P.reset("abf", mb)
        P.reset("af", mf)

    QG = min(512, S)
    TG = QG // 128
    qT_d = nc.dram_tensor("qTd", [NT, 128, 384], BF16, kind="Internal")
    trib = P.static("trib", [128, 128], BF16)
    ntrib = P.static("ntrib", [128, 128], BF16)
    noneb = P.static("noneb", [128, 128], BF16)
    CP(P, "dve", trib, trib[:, :], cstf, cstf[:, C_TRI:C_TRI + 128])
    CP(P, "dve", ntrib, ntrib[:, :], cstf, cstf[:, C_NTRI:C_NTRI + 128])
    CP(P, "dve", noneb, noneb[:, :], cstf, cstf[:, C_NONE:C_NONE + 128])
    trif = Tile(cstf[:, C_TRI:C_TRI + 128], cstf.keys)
    cw = P.static("cw", [128, 6, 5], F32)
    sgb = P.static("sgb", [128, 4], F32)
    Cn = P.static("Cn", [128, 3, 65], F32)
    Cb = P.static("Cb", [128, 3, 65], BF16)
    cvin = P.static("cvin", [128, 6, 131], F32)
    v1 = P.static("v1", [128, 6, 65], BF16)
    sm2 = P.static("sm2", [128, 64], F32)
    ss2 = P.static("ss2", [128, 4], F32)
    gif = P.static("gif", [128, 12], F32)
    nlf = P.static("nlf", [128, 6], F32)
    nb = P.static("nb", [128, 6], F32)
    nhl = P.static("nhl", [128, 2, 6], BF16)
    nh32 = P.static("nh32", [128, 6], F32)
    nbh = P.static("nbh", [128, 2, 6, 128], BF16)
    nbp = P.static("nbp", [128, 2, 3, 128], BF16)
    bD = P.static("bD", [128, 6], F32)
    wsv = P.static("wsv", [128, 6], F32)
    dec = P.static("dec", [128, 3], F32)
    one_t = P.static("one_t", [128, 1], F32)
    P.dve(lambda e: e.memset(one_t[:, :], 1.0), w=[one_t])

    def mix_layer(l, cg=None):
        def pump(n):
            if cg is not None:
                for _ in range(n):
                    next(cg, None)

        mb, mf = P.mark("abf"), P.mark("af")
        wout = P.alloc("abf", (8, 1024))
        kTa = P.alloc("abf", (3, S))
        va = P.alloc("abf", (NT, 384))
        g1t_ = P.alloc("af", (D,))
        ogt_ = P.alloc("af", (D,))
        xt = P.alloc("af", (D,))
        mb1, mf1 = P.mark("abf"), P.mark("af")
        winA = P.alloc("abf", (8, COLS_A))
        winB = P.alloc("abf", (8, 768))
        sgw = P.alloc("abf", (4, 128))
        xn = P.alloc("abf", (D,))
        xnT = P.alloc("abf", (8, 128))
        qkb = P.alloc("abf", (768,))
        mqT = P.alloc("abf", (3, 128))
        mkT = P.alloc("abf", (3, 128))
        qsTz = P.alloc("abf", (6, 128))
        ebp = P.alloc("abf", (3, 128))
        mktok = P.alloc("abf", (384,))
        kw = P.alloc("abf", (384,))
        PT = P.alloc("abf", (6, 128))
        mqTz = Tile(PT.ap, PT.keys)
        og = P.alloc("abf", (384,))
        ymsb = P.alloc("abf", (640,))
        sgvb = P.alloc("abf", (256,))
        qst = P.alloc("abf", (384,))
        zq = P.alloc("af", (768,))
        uv = P.alloc("af", (512,))
        cva = P.alloc("af", (6, 128))
        yb = P.alloc("af", (D,))
        DTf = Tile(cva.ap, cva.keys)

        P.dma("sp", "g1t", g1t_[:, :], bcast_rows(gvec_d, (l * 3 + 0) * D, D), w=[g1t_])
        P.dma("sp", "ogt", ogt_[:, :], bcast_rows(gvec_d, (l * 3 + 2) * D, D), w=[ogt_])
        P.dma("sp", "svt", svt[:, :], bcast_rows(svec_d, l * 396, 396), w=[svt])
        P.dma("sp", "cw", cw.ap.rearrange("p a b -> p (a b)"), convw_d.ap()[l], w=[cw])
        P.dma("sp", "sgb", sgb[:, :], sgb_d.ap()[l], w=[sgb])
        load_cast(winA, winA.ap.rearrange("p a b -> p (a b)"), wina_d.ap()[l], 8 * COLS_A)
        load_cast(winB, winB.ap.rearrange("p a b -> p (a b)"), winb_d.ap()[l], 8 * 768)
        load_cast(wout, wout.ap.rearrange("p a b -> p (a b)"), wout_d.ap()[l], 8 * 1024)
        P.dma("sp", "stg0", stg[0][:, 0:512], sgw_d.ap()[l], w=[stg[0]])
        TT(P, "dve", sgw, sgw[:, :, :], stg[0], stg[0][:, 0:512].rearrange("p (g t) -> p g t", g=4), cstf,
           bc(cstf[:, C_TRI:C_TRI + 128], 1, 4), ALU.mult)
        P.dve(lambda e: e.memset(Cn[:, :, :], 0.0), w=[Cn])
        P.dve(lambda e: e.memset(Cb[:, :, :], 0.0), w=[Cb])
        P.dve(lambda e: e.memset(cvin[:, :, 0:3], 0.0), w=[cvin])
        P.dve(lambda e: e.memset(v1[:, :, 64:65], 1.0), w=[v1])

        chunks = [(0, 512), (512, 512), (1024, 512), (1536, 512), (2048, 396)]
        for tt in range(NT):
            tsl = slice(tt * 128, (tt + 1) * 128)
            P.dma("sp", "xt", xt[:, :], out_d.ap()[tsl, :], r=[dx(tt)], w=[xt])
            norm_transpose(xt, g1t_, xn, xnT, xnT[:, :, :], ss2, sm2)
            pump(max(1, 160 // NT))
            for ci, (c0, cw_) in enumerate(chunks):
                b = ps[ci % 2]
                for kc in range(8):
                    MM(P, b, b[:, 0:cw_], xnT, xnT[:, kc, :], winA, winA[:, kc, c0:c0 + cw_], start=(kc == 0), stop=(kc == 7))
                if ci == 0:
                    CP(P, "act", zq, zq[:, 0:512], b, b[:, 0:512])
                elif ci == 1:
                    CP(P, "act", zq, zq[:, 512:768], b, b[:, 0:256])
                    CP(P, "act", va, va[:, tt, 0:256], b, b[:, 256:512])
                elif ci == 2:
                    CP(P, "act", va, va[:, tt, 256:384], b, b[:, 0:128])
                    CP(P, "act", v1, v1[:, :, 0:64], b, b[:, 128:512].rearrange("p (h d) -> p h d", h=6))
                elif ci == 3:
                    ACTV(P, og, og[:, :], b, b[:, 0:384], AF.Sigmoid)
                    ACTV(P, uv, uv[:, 0:128], b, b[:, 384:512], AF.Gelu_apprx_tanh)
                else:
                    ACTV(P, uv, uv[:, 128:512], b, b[:, 0:384], AF.Gelu_apprx_tanh)
                    TT(P, "dve", gif, gif[:, :], b, b[:, 384:396], svt, svt[:, 384:396], ALU.add)
            P.pos("zT")
            for c in range(6):
                b = ps[2] if c < 4 else ps[3]
                o_ = (c % 4) * 128
                for kc in range(8):
                    MM(P, b, b[:, o_:o_ + 128], winB, winB[:, kc, c * 128:(c + 1) * 128], xnT, xnT[:, kc, :],
                       start=(kc == 0), stop=(kc == 7))
            CP(P, "act", cvin, cvin[:, 0:4, 3:131], ps[2], ps[2][:, :].rearrange("p (a b) -> p a b", a=4))
            CP(P, "act", cvin, cvin[:, 4:6, 3:131], ps[3], ps[3][:, 0:256].rearrange("p (a b) -> p a b", a=2))
            P.pos("sbnorm")
            z12 = zq.ap.rearrange("p (h d) -> p h d", h=12)
            y12 = yb.ap[:, 0:768].rearrange("p (h d) -> p h d", h=12)
            TT(P, "dve", yb, y12, zq, z12, zq, z12, ALU.mult)
            RED(P, sm2, sm2[:, 0:12], yb, y12, ALU.add)
            rstd_of(sm2, sm2[:, 0:12], 1.0 / 64, sm2, sm2[:, 16:28], sm2, sm2[:, 32:44])
            TS(P, "dve", sm2, sm2[:, 16:22], sm2, sm2[:, 16:22], 0.125, None, ALU.mult)
            TT(P, "dve", zq, z12, zq, z12, sm2, bc(sm2[:, 16:28], 2, 64), ALU.mult)
            gqk = AP(svt.ap.tensor, svt.ap.offset, [list(svt.ap.ap[0]), [64, 2], [0, 6], [1, 64]])
            TT(P, "dve", qkb, qkb.ap.rearrange("p (c h d) -> p c h d", c=2, h=6), zq,
               zq.ap.rearrange("p (c h d) -> p c h d", c=2, h=6), svt, gqk, ALU.mult)
            for p_ in range(6):
                TR(P, psT, psT[:, p_ * 128:(p_ + 1) * 128], qkb, qkb[:, p_ * 128:(p_ + 1) * 128], identb, identb[:, :])
            CP(P, "act", qst, qst[:, :], psT, psT[:, 0:384])
            P.dma("pool", "qst", qT_d.ap()[tt], qst[:, :], r=[qst], w=[P.dkey("qT", tt)])
            CP(P, "act", kTa, kTa[:, :, tsl], psT, psT[:, 384:768].rearrange("p (a b) -> p a b", a=3))
            P.pos("conv")
            y6 = yb.ap[:, 0:768].rearrange("p (c t) -> p c t", c=6)
            TT(P, "dve", cva, cva[:, :, :], cvin, cvin[:, :, 0:128], cw, bc(cw[:, :, 0], 2, 128), ALU.mult)
            TT(P, "dve", cva, cva[:, :, :], cva, cva[:, :, :], cw, bc(cw[:, :, 4], 2, 128), ALU.add)
            for j in range(1, 4):
                TT(P, "dve", yb, y6, cvin, cvin[:, :, j:j + 128], cw, bc(cw[:, :, j], 2, 128), ALU.mult)
                TT(P, "dve", cva, cva[:, :, :], cva, cva[:, :, :], yb, y6, ALU.add)
            CP(P, "dve", cvin, cvin[:, :, 0:3], cvin, cvin[:, :, 128:131])
            ACTV(P, yb, y6, cva, cva[:, :, :], AF.Sigmoid)
            TT(P, "dve", mqT, mqT[:, :, :], cva, cva[:, 0:3, :], yb, y6[:, 0:3, :], ALU.mult)
            STT(P, mkT, mkT[:, :, :], cva, cva[:, 3:6, :], 0.125, yb, y6[:, 3:6, :], ALU.mult, ALU.mult)
            for p_ in range(3):
                TR(P, psT, psT[:, p_ * 128:(p_ + 1) * 128], mkT, mkT[:, p_, :], identb, identb[:, :])
            CP(P, "act", mktok, mktok[:, :], psT, psT[:, 0:384])
            P.pos("gates")
            ACTV(P, nlf, nlf[:, :], gif, gif[:, 6:12], AF.Exp, scale=-1.0)
            ACTV(P, nlf, nlf[:, :], nlf, nlf[:, :], AF.Ln, bias=one_t[:, 0:1], r=[one_t])
            b6 = ps[6]
            CP(P, "dve", nhl, nhl[:, 0, :], nlf, nlf[:, :])
            CP(P, "dve", nh32, nh32[:, :], nhl, nhl[:, 0, :])
            TT(P, "dve", nhl, nhl[:, 1, :], nlf, nlf[:, :], nh32, nh32[:, :], ALU.subtract)
            CP(P, "dve", nbh, nbh[:, :, :, :], nhl, bc(nhl[:, :, :], 3, 128))
            CP(P, "dve", nbp, nbp.ap.rearrange("p c a (b d) -> p (c a) b d", b=2), nhl,
               bc(nhl.ap.rearrange("p c (a b) -> p (c a) b", a=3), 3, 64))
            for h in range(6):
                b = ps[4] if h < 4 else ps[5]
                o_ = (h % 4) * 128
                for c_ in range(2):
                    MM(P, b, b[:, o_:o_ + 128], nbh, nbh[:, c_, h, :], trib, trib[:, :], start=(c_ == 0), stop=(c_ == 1))
            for p_ in range(3):
                for c_ in range(2):
                    MM(P, b6, b6[:, p_ * 128:(p_ + 1) * 128], nbp, nbp[:, c_, p_, :], trib, trib[:, :], start=(c_ == 0), stop=(c_ == 1))
            yd = yb.ap[:, 0:768].rearrange("p (h t) -> p h t", h=6)
            TT(P, "dve", yb, yd[:, 0:4, :], ps[4], ps[4][:, :].rearrange("p (a b) -> p a b", a=4), cstf, bc(cstf[:, C_IDF:C_IDF + 128], 1, 4), ALU.mult)
            TT(P, "dve", yb, yd[:, 4:6, :], ps[5], ps[5][:, 0:256].rearrange("p (a b) -> p a b", a=2), cstf, bc(cstf[:, C_IDF:C_IDF + 128], 1, 2), ALU.mult)
            RED(P, nb, nb[:, :], yb, yd, ALU.add)
            TT(P, "dve", bD, bD[:, :], nb, nb[:, :], gif, gif[:, 0:6], ALU.add)
            for h in range(6):
                b = ps[4] if h < 4 else ps[5]
                o_ = (h % 4) * 128
                ACTV(P, DTf, DTf[:, h, :], b, b[:, o_:o_ + 128], AF.Exp, bias=bD[:, h:h + 1], scale=-1.0, r=[bD])
            TT(P, "dve", DTf, DTf[:, :, :], DTf, DTf[:, :, :], cstf, bc(cstf[:, C_TRI:C_TRI + 128], 1, 6), ALU.mult)
            ACTV(P, ebp, ebp[:, :, :], b6, b6[:, 0:384].rearrange("p (a b) -> p a b", a=3), AF.Exp, scale=-1.0)
            ACTV(P, dec, dec[:, :], b6, b6[:, 0:384].rearrange("p (a b) -> p a b", a=3)[:, :, 127], AF.Exp, scale=-1.0)
            TT(P, "dve", wsv, wsv[:, 0:4], bD, bD[:, 0:4], ps[4], ps[4][:, :].rearrange("p (a b) -> p a b", a=4)[:, :, 127],
               ALU.subtract)
            TT(P, "dve", wsv, wsv[:, 4:6], bD, bD[:, 4:6], ps[5], ps[5][:, 0:256].rearrange("p (a b) -> p a b", a=2)[:, :, 127],
               ALU.subtract)
            ACTV(P, wsv, wsv[:, :], wsv, wsv[:, :], AF.Exp)
            P.dve(lambda e: e.memset(mqTz[:, :, :], 0.0), w=[mqTz])
            mz4 = mqTz.ap.rearrange("p (a b) t -> p a b t", b=2)
            CP(P, "dve", mqTz, mz4[0:64, :, 0, :], mqT, mqT[0:64, :, :])
            CP(P, "dve", mqTz, mz4[64:128, :, 1, :], mqT, mqT[64:128, :, :])
            TT(P, "dve", qsTz, qsTz.ap.rearrange("p (a b) t -> p a b t", b=2), mqTz, mz4, ebp, bc(ebp[:, :, :], 2, 2), ALU.mult)
            TT(P, "dve", kw, kw.ap.rearrange("p (h d) -> p h d", h=6), mktok, mktok.ap.rearrange("p (h d) -> p h d", h=6),
               wsv, bc(wsv[:, :], 2, 64), ALU.mult)
            P.pos("ST")
            for h in range(6):
                b = ps[2] if h < 4 else ps[3]
                o_ = (h % 4) * 128
                p_, r0 = h // 2, (h % 2) * 64
                MM(P, b, b[:, o_:o_ + 128], mkT, mkT[:, p_, :], mqTz, mqTz[:, h, :])
            TT(P, "dve", PT, PT[:, 0:4, :], ps[2], ps[2][:, :].rearrange("p (a b) -> p a b", a=4), DTf, DTf[:, 0:4, :], ALU.mult)
            TT(P, "dve", PT, PT[:, 4:6, :], ps[3], ps[3][:, 0:256].rearrange("p (a b) -> p a b", a=2), DTf, DTf[:, 4:6, :], ALU.mult)
            P.pos("numden")
            nd = ps[0]
            for h in range(6):
                p_, r0 = h // 2, (h % 2) * 64
                MM(P, nd, nd[:, h * 65:(h + 1) * 65], PT, PT[:, h, :], v1, v1[:, h, :], start=True, stop=(tt == 0))
                if tt > 0:
                    MM(P, nd, nd[:, h * 65:(h + 1) * 65], qsTz, qsTz[:, h, :], Cb, Cb[:, p_, :], start=False, stop=True)
            nd3 = nd[:, 0:390].rearrange("p (h e) -> p h e", h=6)
            CP(P, "dve", sm2, sm2[:, 40:46], nd, nd3[:, :, 64])
            TS(P, "dve", sm2, sm2[:, 48:54], sm2, sm2[:, 40:46], -1.0, None, ALU.mult)
            TT(P, "dve", sm2, sm2[:, 48:54], sm2, sm2[:, 48:54], sm2, sm2[:, 40:46], ALU.max)
            TS(P, "dve", sm2, sm2[:, 48:54], sm2, sm2[:, 48:54], 1.0, None, ALU.max)
            P.dve(lambda e: e.reciprocal(out=sm2[:, 56:62], in_=sm2[:, 48:54]), r=[sm2], w=[sm2])
            TT(P, "dve", yb, yb[:, 384:768].rearrange("p (h d) -> p h d", h=6), nd, nd3[:, :, 0:64], sm2, bc(sm2[:, 56:62], 2, 64),
               ALU.mult)
            P.pos("state")
            su = ps[1]
            for p_ in range(3):
                for s_ in range(2):
                    h = 2 * p_ + s_
                    MM(P, su, su[:, h * 65:(h + 1) * 65], kw, kw[:, p_ * 128:(p_ + 1) * 128], v1, v1[:, h, :])
            for p_ in range(3):
                for s_ in range(2):
                    h = 2 * p_ + s_
                    r0 = s_ * 64
                    STT(P, Cn, Cn[r0:r0 + 64, p_, :], Cn, Cn[r0:r0 + 64, p_, :], dec[r0:r0 + 64, p_:p_ + 1], su,
                        su[r0:r0 + 64, h * 65:(h + 1) * 65], ALU.mult, ALU.add, r=[dec])
            CP(P, "dve", Cb, Cb[:, :, :], Cn, Cn[:, :, :])
            P.pos("sg")
            u4 = uv.ap[:, 0:256].rearrange("p (g d) -> p g d", g=4)
            v4_ = uv.ap[:, 256:512].rearrange("p (g d) -> p g d", g=4)
            z4 = zq.ap[:, 0:256].rearrange("p (g d) -> p g d", g=4)
            TT(P, "dve", zq, z4, uv, v4_, uv, v4_, ALU.mult)
            RED(P, sm2, sm2[:, 0:4], zq, z4, ALU.add)
            rstd_of(sm2, sm2[:, 0:4], 1.0 / 64, sm2, sm2[:, 16:20], sm2, sm2[:, 32:36])
            TT(P, "dve", uv, v4_, uv, v4_, sm2, bc(sm2[:, 16:20], 2, 64), ALU.mult)
            TT(P, "dve", sgvb, sgvb[:, :], uv, uv[:, 256:512], svt, svt[:, 128:384], ALU.mult)
            gb = ps[5]
            for g in range(4):
                MM(P, gb, gb[:, 256 + g * 64:256 + (g + 1) * 64], sgw, sgw[:, g, :], sgvb, sgvb[:, g * 64:(g + 1) * 64])
            y4 = yb.ap[:, 768:1024].rearrange("p (g d) -> p g d", g=4)
            TT(P, "dve", yb, y4, gb, gb[:, 256:512].rearrange("p (g d) -> p g d", g=4), sgb, bc(sgb[:, :], 2, 64), ALU.add)
            TT(P, "dve", yb, y4, yb, y4, uv, u4, ALU.mult)
            P.pos("onorm")
            y10 = yb.ap[:, 384:1024].rearrange("p (h d) -> p h d", h=10)
            z10 = zq.ap[:, 0:640].rearrange("p (h d) -> p h d", h=10)
            TT(P, "dve", zq, z10, yb, y10, yb, y10, ALU.mult)
            RED(P, sm2, sm2[:, 0:10], zq, z10, ALU.add)
            rstd_of(sm2, sm2[:, 0:10], 1.0 / 64, sm2, sm2[:, 16:26], sm2, sm2[:, 32:42])
            TT(P, "dve", yb, y10, yb, y10, sm2, bc(sm2[:, 16:26], 2, 64), ALU.mult)
            TT(P, "dve", yb, yb[:, 384:1024], yb, yb[:, 384:1024], ogt_, ogt_[:, 384:1024], ALU.mult)
            TT(P, "dve", ymsb, ymsb[:, 0:384], yb, yb[:, 384:768], og, og[:, :], ALU.mult)
            CP(P, "act", ymsb, ymsb[:, 384:640], yb, yb[:, 768:1024])
            P.dma("pool", "ymsb", yms_d.ap()[tsl, :], ymsb[:, :], r=[ymsb], w=[P.dkey("yms", tt)])
        P.reset("abf", mb1)
        P.reset("af", mf1)
        P.pos("M2")
        mask4b = P.alloc("abf", (4, 512))
        y_bf = P.alloc("abf", (D,))
        yT = P.alloc("abf", (8, 128))
        qTg = P.alloc("abf", (3, QG))
        qTgz = P.alloc("abf", (6, QG))
        spb = [P.alloc("abf", (QG,)) for _ in range(2)]
        Lacc = [P.alloc("abf", (QG,)) for _ in range(2)]
        wT = [P.alloc("abf", (QG,)) for _ in range(2)]
        e32 = [P.alloc("af", (QG,)) for _ in range(2)]
        ysb = P.alloc("af", (TG, 384))
        sqb = P.alloc("af", (384,))
        yfs = P.alloc("af", (QG,))
        for r_ in range(4):
            P.dma("sp", "stg1", stg[1][:, 0:512], cst_d.ap()[:, C_MASK4 + r_ * 512:C_MASK4 + (r_ + 1) * 512], w=[stg[1]])
            CP(P, "dve", mask4b, mask4b[:, r_, :], stg[1], stg[1][:, 0:512])
        for g in range(S // QG):
            for i in range(TG):
                P.dma("sp", "qTg", qTg[:, :, i * 128:(i + 1) * 128], qT_d.ap()[g * TG + i].rearrange("p (a b) -> p a b", a=3),
                      r=[P.dkey("qT", g * TG + i)], w=[qTg])
            nkb = (g + 1) * TG
            P.dve(lambda e: e.memset(qTgz[:, :, :], 0.0), w=[qTgz])
            qz4 = qTgz.ap.rearrange("p (a b) t -> p a b t", b=2)
            CP(P, "dve", qTgz, qz4[0:64, :, 0, :], qTg, qTg[0:64, :, :])
            CP(P, "dve", qTgz, qz4[64:128, :, 1, :], qTg, qTg[64:128, :, :])
            for h in range(6):
                p_, r0 = h // 2, (h % 2) * 64
                ya = ps[4 + h % 2]
                pump(max(1, 120 // (6 * (S // QG))))
                it = 0
                for kb in range(nkb - 1, -1, -1):
                    rr = kb - g * TG
                    first = (kb == nkb - 1)
                    zb = ps[it % 2]
                    Bb = ps[2 + it % 2]
                    kap = kTa[:, p_, kb * 128:(kb + 1) * 128]
                    qap = qTgz[:, h, :]
                    MM(P, zb, zb[:, 0:QG], kTa, kap, qTgz, qap)
                    e_ = e32[it % 2]
                    s_ = spb[it % 2]
                    w_ = wT[it % 2]
                    ACTV(P, e_, e_[:, :], zb, zb[:, 0:QG], AF.Exp)
                    ACTV(P, s_, s_[:, :], e_, e_[:, :], AF.Ln, bias=one_t[:, 0:1], r=[one_t])
                    if rr >= 0:
                        TT(P, "dve", s_, s_[:, :], s_, s_[:, :], mask4b, mask4b[:, rr, 0:QG], ALU.mult)
                    MM(P, Bb, Bb[:, 0:QG], kTa, kap, qTgz, qap, start=True, stop=False)
                    MM(P, Bb, Bb[:, 0:QG], ntrib, ntrib[:, :], s_, s_[:, :], start=False, stop=first)
                    if not first:
                        MM(P, Bb, Bb[:, 0:QG], noneb, noneb[:, :], Lacc[it % 2], Lacc[it % 2][:, :], start=False, stop=True)
                    ACTV(P, w_, w_[:, :], Bb, Bb[:, 0:QG], AF.Exp)
                    if rr >= 0:
                        TT(P, "dve", w_, w_[:, :], w_, w_[:, :], mask4b, mask4b[:, rr, 0:QG], ALU.mult)
                    if kb > 0:
                        if first:
                            CP(P, "dve", Lacc[(it + 1) % 2], Lacc[(it + 1) % 2][:, :], s_, s_[:, :])
                        else:
                            TT(P, "dve", Lacc[(it + 1) % 2], Lacc[(it + 1) % 2][:, :], Lacc[it % 2], Lacc[it % 2][:, :], s_, s_[:, :],
                               ALU.add)
                    MM(P, ya, ya[:, 0:QG], va, va[:, kb, p_ * 128:(p_ + 1) * 128], w_, w_[:, :], start=first, stop=(kb == 0))
                    it += 1
                CP(P, "act", yfs, yfs[:, :], ya, ya[:, 0:QG])
                tb_ = ps[6]
                for qt in range(TG):
                    TR(P, tb_, tb_[:, qt * 128:(qt + 1) * 128], yfs, yfs[:, qt * 128:(qt + 1) * 128], identf, identf[:, :])
                CP(P, "act", ysb, ysb.ap.rearrange("p t (h d) -> p t h d", h=6)[:, :, h, :], tb_,
                   tb_[:, 0:TG * 128].rearrange("p (t d) -> p t d", t=TG)[:, :, r0:r0 + 64])
            for qt in range(TG):
                tt = g * TG + qt
                tsl = slice(tt * 128, (tt + 1) * 128)
                y6_ = ysb.ap[:, qt, :].rearrange("p (h d) -> p h d", h=6)
                s6_ = sqb.ap.rearrange("p (h d) -> p h d", h=6)
                TT(P, "dve", sqb, s6_, ysb, y6_, ysb, y6_, ALU.mult)
                RED(P, sm2, sm2[:, 0:6], sqb, s6_, ALU.add)
                rstd_of(sm2, sm2[:, 0:6], 1.0 / 64, sm2, sm2[:, 16:22], sm2, sm2[:, 32:38])
                TT(P, "dve", sqb, s6_, ysb, y6_, sm2, bc(sm2[:, 16:22], 2, 64), ALU.mult)
                TT(P, "dve", y_bf, y_bf[:, 0:384], sqb, sqb[:, :], ogt_, ogt_[:, 0:384], ALU.mult)
                P.dma("sp", "ybf", y_bf[:, 384:1024], yms_d.ap()[tsl, :], r=[P.dkey("yms", tt)], w=[y_bf])
                for kc in range(8):
                    TR(P, psT, psT[:, kc * 128:(kc + 1) * 128], y_bf, y_bf[:, kc * 128:(kc + 1) * 128], identb, identb[:, :])
                CP(P, "act", yT, yT[:, :, :], psT, psT[:, :].rearrange("p (a b) -> p a b", a=8))
                P.dma("sp", "xt", xt[:, :], out_d.ap()[tsl, :], r=[dx(tt)], w=[xt])
                for hf in range(2):
                    ob = ps[6] if hf == 0 else ps[0]
                    for kc in range(8):
                        MM(P, ob, ob[:, :], yT, yT[:, kc, :], wout, wout[:, kc, hf * 512:(hf + 1) * 512], start=(kc == 0), stop=(kc == 7))
                    TT(P, "dve", xt, xt[:, hf * 512:(hf + 1) * 512], xt, xt[:, hf * 512:(hf + 1) * 512], ob, ob[:, :], ALU.add)
                P.dma("pool", "stxm", out_d.ap()[tsl, :], xt[:, :], r=[xt], w=[dx(tt)])
        P.reset("abf", mb)
        P.reset("af", mf)

    for l in range(L):
        cg = ffn_cast_gen(l) if "ffn" in phases else None
        if "mix" in phases:
            mix_layer(l, cg)
        if "ffn" in phases:
            for _ in cg:
                pass
            ffn_layer(l)

    P.finalize()
    return nc, es, P


def prep_weights(inp, L, ffn=True):
    f = lambda a: np.ascontiguousarray(np.asarray(a, dtype=np.float32))
    w = {}
    w["cst"] = make_consts()
    w["gvec"] = f(np.stack([inp["norm1_g"][:L], inp["norm2_g"][:L], inp["out_g"][:L]], axis=1))
    w["svec"] = f(np.concatenate([inp["sb_qn_g"][:L], inp["sb_kn_g"][:L], inp["sg_vn_g"][:L], inp["ml_i_b"][:L],
                                  inp["ml_f_b"][:L]], axis=1))
    cw = np.asarray(inp["ml_conv_w"][:L]).reshape(L, 4, 6, 128).transpose(0, 3, 2, 1)
    cb = np.asarray(inp["ml_conv_b"][:L]).reshape(L, 6, 128).transpose(0, 2, 1)[..., None]
    w["convw"] = f(np.concatenate([cw, cb], axis=3).reshape(L, 128, 30))
    w["sgw"] = f(np.asarray(inp["sg_w"][:L]).transpose(0, 3, 1, 2).reshape(L, 128, 512))
    w["sgb"] = f(np.asarray(inp["sg_b"][:L]).transpose(0, 2, 1))
    win = np.asarray(inp["w_in"][:L])
    cols_a = np.concatenate([np.arange(0, 1152), np.arange(1920, 2688), np.arange(2700, 3212), np.arange(2688, 2700)])
    cols_b = np.arange(1152, 1920)

    def kmaj(m):
        n = m.shape[2]
        return f(m.reshape(L, 8, 128, n).transpose(0, 2, 1, 3).reshape(L, 128, 8 * n))
    w["wina"] = kmaj(win[:, :, cols_a])
    w["winb"] = kmaj(win[:, :, cols_b])
    w["wout"] = kmaj(np.asarray(inp["w_out"][:L]))
    w["wq"] = kmaj(np.asarray(inp["peer_wq"][:L]))
    w["pkeys"] = f(np.asarray(inp["peer_keys"][:L]).reshape(L, 16, 128, 128).transpose(0, 3, 1, 2).reshape(L, 128, 2048))
    if not ffn:
        return w
    u = np.asarray(inp["peer_u"][:L]).reshape(L, 128, 128, 8, 128)
    w["ut"] = f(u.transpose(0, 2, 4, 3, 1).reshape(L, 128, 128, 1024))
    v = np.asarray(inp["peer_v"][:L]).reshape(L, 128, 128, 1024)
    w["vt"] = f(v.transpose(0, 2, 1, 3))
    return w


_CACHE = {}


def run(inputs, S, L, phases=("mix", "ffn"), ncores=NCORES):
    x = np.asarray(inputs["x"], dtype=np.float32)
    key = (S, L, tuple(phases))
    if key not in _CACHE:
        _CACHE[key] = build(S, L, phases)
    nc, es, P = _CACHE[key]
    w = prep_weights(inputs, L, "ffn" in phases)
    in_maps = []
    for c in range(ncores):
        m = dict(w)
        m["x"] = np.ascontiguousarray(x[c, :S])
        in_maps.append(m)
    res = run_bass_kernel_spmd(nc, in_maps, core_ids=list(range(ncores)))
    return np.stack([np.asarray(r["out"]) for r in res.results], axis=0)


def kernel(**inputs):
    return run(inputs, 4096, 4).astype(np.float32)
```
